# Optimizing a Trainium2 kernel written in Bass

```python
import math
import jax, jax.numpy as jnp
from jax import lax
import numpy as np

D_MODEL = 4096
BATCH = 4
SEQ = 4096
DEPTH = 1

N_META = 16
D_S5 = D_MODEL // 2
S5_GROUP = 16
S5_GROUPS = D_S5 // S5_GROUP
S5_STATE = 64
D_HG = D_MODEL // 2
HG_EXPAND = 128
HG_HEADS = D_HG // HG_EXPAND
HG_CHUNK = 64
N_EXPERT_GROUPS = 8
EXPERTS_PER_GROUP = 8
N_EXPERTS = N_EXPERT_GROUPS * EXPERTS_PER_GROUP
TOP_K_INNER = 2
D_EXPERT = D_MODEL // 8
MOE_BLOCK = 128
RMS_EPS = 1e-6
IN_SIZES = (D_S5, D_HG, D_HG, D_HG, D_HG, D_HG, D_MODEL, D_MODEL)
IN_COLS = sum(IN_SIZES)
IN_SPLITS = tuple(int(s) for s in np.cumsum(IN_SIZES)[:-1])

kernel_name = "hybrid_s5_hgrn2_hmoe_encoder"


def rmsnorm(x, w):
    xf = x.astype(jnp.float32)
    y = xf * lax.rsqrt(jnp.mean(xf * xf, axis=-1, keepdims=True) + RMS_EPS)
    return (y * w.astype(jnp.float32)).astype(x.dtype)


def cmul(ar, ai, br, bi):
    return ar * br - ai * bi, ar * bi + ai * br


def s5_direction(u, a_re, a_im, log_dt, b_re, b_im, c_re, c_im, reverse):
    dt = jnp.exp(log_dt)[:, None]
    mag = jnp.exp(a_re * dt)
    ab_re, ab_im = mag * jnp.cos(a_im * dt), mag * jnp.sin(a_im * dt)
    den = a_re * a_re + a_im * a_im
    nr, ni = ab_re - 1.0, ab_im
    z_re = (nr * a_re + ni * a_im) / den
    z_im = (ni * a_re - nr * a_im) / den
    bb_re, bb_im = cmul(z_re[..., None], z_im[..., None], b_re, b_im)
    bu_re = jnp.einsum('blgh,gph->blgp', u, bb_re)
    bu_im = jnp.einsum('blgh,gph->blgp', u, bb_im)
    L = u.shape[1]
    a_r = jnp.broadcast_to(ab_re, (1, L) + ab_re.shape)
    a_i = jnp.broadcast_to(ab_im, (1, L) + ab_im.shape)

    def combine(e1, e2):
        a1r, a1i, b1r, b1i = e1
        a2r, a2i, b2r, b2i = e2
        ar, ai = cmul(a2r, a2i, a1r, a1i)
        tr, ti = cmul(a2r, a2i, b1r, b1i)
        return ar, ai, tr + b2r, ti + b2i

    _, _, xr, xi = lax.associative_scan(combine, (a_r, a_i, bu_re, bu_im), reverse=reverse, axis=1)
    return jnp.einsum('blgp,ghp->blgh', xr, c_re) - jnp.einsum('blgp,ghp->blgh', xi, c_im)


def gla_chunkwise(q, k, v, logg):
    B, H, T, K = q.shape
    V = v.shape[-1]
    n = T // HG_CHUNK

    def to_chunks(t):
        return jnp.moveaxis(t.reshape(B, H, n, HG_CHUNK, t.shape[-1]), 2, 0)

    incl = jnp.tril(jnp.ones((HG_CHUNK, HG_CHUNK), dtype=bool))[:, :, None]

    def step(S, inp):
        qc, kc, vc, gc = inp
        b = jnp.cumsum(gc, axis=2)
        b_last = b[:, :, -1:, :]
        o_inter = jnp.einsum('bhtk,bhkv->bhtv', qc * jnp.exp(b), S)
        decay = jnp.exp(jnp.where(incl, b[:, :, :, None, :] - b[:, :, None, :, :], -jnp.inf))
        scores = jnp.einsum('bhtk,bhsk,bhtsk->bhts', qc, kc, decay)
        o_intra = jnp.einsum('bhts,bhsv->bhtv', scores, vc)
        S = jnp.exp(b_last[:, :, 0, :])[..., None] * S + jnp.einsum('bhsk,bhsv->bhkv', kc * jnp.exp(b_last - b), vc)
        return S, o_inter + o_intra

    S0 = jnp.zeros((B, H, K, V), jnp.float32)
    _, o = lax.scan(step, S0, (to_chunks(q), to_chunks(k), to_chunks(v), to_chunks(logg)))
    return jnp.moveaxis(o, 0, 2).reshape(B, H, T, V)


def hybrid_mixer(a, layer, w_in, s5_a_re, s5_a_im, s5_log_dt, s5_b_re, s5_b_im, s5_c_re, s5_c_im,
                 s5_d, s5_glu_w, s5_glu_b, hg_lb_gamma, hg_norm_w, w_branch_a, w_branch_b, w_out):
    B, L, _ = a.shape
    f32 = jnp.float32
    proj = jnp.einsum('bld,de->ble', a, w_in)
    u, q, i_in, f_fw, f_bw, g_out, gate_a, gate_b = jnp.split(proj, IN_SPLITS, axis=-1)

    uf = u.astype(f32).reshape(B, L, S5_GROUPS, S5_GROUP)
    y = uf.reshape(B, L, D_S5) * s5_d.astype(f32)
    for d in range(2):
        y = y + s5_direction(uf, s5_a_re[d].astype(f32), s5_a_im[d].astype(f32), s5_log_dt[d].astype(f32),
                             s5_b_re[d].astype(f32), s5_b_im[d].astype(f32), s5_c_re[d].astype(f32),
                             s5_c_im[d].astype(f32), reverse=(d == 1)).reshape(B, L, D_S5)
    z = jax.nn.gelu(y)
    s5_out = z * jax.nn.sigmoid(z @ s5_glu_w.astype(f32) + s5_glu_b.astype(f32))

    lb = jnp.cumsum(jax.nn.softmax(hg_lb_gamma.astype(f32), axis=1), axis=1)[:, layer]
    n_pad = HG_CHUNK - N_META

    def heads(t):
        return t.reshape(B, L, HG_HEADS, HG_EXPAND).transpose(0, 2, 1, 3)

    def pad(t):
        return jnp.pad(t, ((0, 0), (0, 0), (n_pad, 0), (0, 0)))

    qh = pad(heads(jax.nn.silu(q.astype(f32))))
    vh = pad(heads(i_in.astype(f32)))
    o = jnp.zeros(qh.shape, f32)
    for d, f_logit in enumerate((f_fw, f_bw)):
        f = lb[d] + (1.0 - lb[d]) * jax.nn.sigmoid(f_logit.astype(f32))
        kh, gh = pad(heads(1.0 - f)), pad(heads(jnp.log(f)))
        if d == 0:
            o = o + gla_chunkwise(qh, kh, vh, gh)
        else:
            o = o + jnp.flip(gla_chunkwise(jnp.flip(qh, 2), jnp.flip(kh, 2), jnp.flip(vh, 2), jnp.flip(gh, 2)), 2)
    o = o[:, :, n_pad:]
    o = o * lax.rsqrt(jnp.mean(o * o, axis=-1, keepdims=True) + RMS_EPS)
    o = o.transpose(0, 2, 1, 3).reshape(B, L, D_HG)
    hg_out = o * hg_norm_w.astype(f32) * jax.nn.silu(g_out.astype(f32))

    y_a = jnp.einsum('blc,cd->bld', s5_out.astype(a.dtype), w_branch_a)
    y_b = jnp.einsum('blc,cd->bld', hg_out.astype(a.dtype), w_branch_b)
    m = jax.nn.sigmoid(gate_a) * y_a + jax.nn.sigmoid(gate_b) * y_b
    return jnp.einsum('bld,de->ble', m, w_out)


def expert_dispatch(xf, e_idx, gate, w1, w3, w2):
    N, K = e_idx.shape
    E = w1.shape[0]
    M = N * K
    flat_e = e_idx.reshape(M)
    flat_tok = jnp.repeat(jnp.arange(N, dtype=jnp.int32), K)
    flat_w = gate.reshape(M)
    order = jnp.argsort(flat_e, stable=True)
    se, stok, sw = flat_e[order], flat_tok[order], flat_w[order]
    counts = jnp.bincount(flat_e, length=E)
    padded = ((counts + MOE_BLOCK - 1) // MOE_BLOCK) * MOE_BLOCK
    pad_end = jnp.cumsum(padded)
    pad_start = pad_end - padded
    grp_start = jnp.cumsum(counts) - counts
    dest = pad_start[se] + jnp.arange(M, dtype=jnp.int32) - grp_start[se]
    n_blocks = -(-M // MOE_BLOCK) + E
    P = n_blocks * MOE_BLOCK
    buf_tok = jnp.zeros((P,), jnp.int32).at[dest].set(stok)
    buf_w = jnp.zeros((P,), jnp.float32).at[dest].set(sw)
    blk_e = jnp.minimum(jnp.searchsorted(pad_end, jnp.arange(n_blocks) * MOE_BLOCK, side='right'), E - 1)

    def run_block(args):
        tok, w, e = args
        xb = xf[tok]
        hb = jax.nn.silu(xb @ w1[e]) * (xb @ w3[e])
        return (hb @ w2[e]) * w[:, None].astype(xf.dtype)

    yb = lax.map(run_block, (buf_tok.reshape(n_blocks, MOE_BLOCK), buf_w.reshape(n_blocks, MOE_BLOCK), blk_e))
    return jnp.zeros_like(xf).at[buf_tok].add(yb.reshape(P, -1))


def hierarchical_moe(h, rg_w, rg_b, re_w, re_b, w1, w3, w2):
    B, L, D = h.shape
    xf = h.reshape(B * L, D)
    hf = xf.astype(jnp.float32)
    p_group = jax.nn.softmax(hf @ rg_w.astype(jnp.float32) + rg_b.astype(jnp.float32), axis=-1)
    g_prob, g_sel = lax.top_k(p_group, 1)
    logits_e = (hf @ re_w.astype(jnp.float32) + re_b.astype(jnp.float32)).reshape(-1, N_EXPERT_GROUPS, EXPERTS_PER_GROUP)
    logits_in = jnp.take_along_axis(logits_e, g_sel[:, :, None], axis=1)[:, 0]
    top_v, top_i = lax.top_k(logits_in, TOP_K_INNER)
    gate = jax.nn.softmax(top_v, axis=-1) * g_prob
    e_idx = g_sel * EXPERTS_PER_GROUP + top_i
    return expert_dispatch(xf, e_idx, gate, w1, w3, w2).reshape(B, L, D)


def setup_inputs(seed: int = 0) -> dict:
    key = jax.random.key(seed)
    ks = jax.random.split(key, 32)
    f32 = jnp.float32

    def nrm(k, shape, scale):
        return jax.random.normal(k, shape, f32) * scale

    G, P, H = S5_GROUPS, S5_STATE, S5_GROUP
    n_idx = jnp.arange(P, dtype=f32)
    return {
        "x": nrm(ks[0], (BATCH, SEQ, D_MODEL), 1.0),
        "meta_tokens": nrm(ks[1], (N_META, D_MODEL), 1.0),
        "norm_mix_w": 1.0 + nrm(ks[2], (DEPTH, D_MODEL), 0.01),
        "w_in": nrm(ks[3], (DEPTH, D_MODEL, IN_COLS), D_MODEL ** -0.5),
        "s5_a_re": -0.5 + nrm(ks[4], (DEPTH, 2, G, P), 0.01),
        "s5_a_im": math.pi * n_idx + nrm(ks[5], (DEPTH, 2, G, P), 0.01),
        "s5_log_dt": jax.random.uniform(ks[6], (DEPTH, 2, G), f32, math.log(1e-3), math.log(1e-1)),
        "s5_b_re": nrm(ks[7], (DEPTH, 2, G, P, H), (2 * H) ** -0.5),
        "s5_b_im": nrm(ks[8], (DEPTH, 2, G, P, H), (2 * H) ** -0.5),
        "s5_c_re": nrm(ks[9], (DEPTH, 2, G, H, P), (2 * P) ** -0.5),
        "s5_c_im": nrm(ks[10], (DEPTH, 2, G, H, P), (2 * P) ** -0.5),
        "s5_d": nrm(ks[11], (DEPTH, D_S5), 1.0),
        "s5_glu_w": nrm(ks[12], (DEPTH, D_S5, D_S5), D_S5 ** -0.5),
        "s5_glu_b": nrm(ks[13], (DEPTH, D_S5), 0.01),
        "hg_lb_gamma": nrm(ks[14], (2, DEPTH + 1, D_HG), 0.1),
        "hg_norm_w": 1.0 + nrm(ks[15], (DEPTH, D_HG), 0.01),
        "w_branch_a": nrm(ks[16], (DEPTH, D_S5, D_MODEL), D_S5 ** -0.5),
        "w_branch_b": nrm(ks[17], (DEPTH, D_HG, D_MODEL), D_HG ** -0.5),
        "w_out": nrm(ks[18], (DEPTH, D_MODEL, D_MODEL), D_MODEL ** -0.5),
        "norm_ffn_w": 1.0 + nrm(ks[19], (DEPTH, D_MODEL), 0.01),
        "router_group_w": nrm(ks[20], (DEPTH, D_MODEL, N_EXPERT_GROUPS), D_MODEL ** -0.5),
        "router_group_b": nrm(ks[21], (DEPTH, N_EXPERT_GROUPS), 0.01),
        "router_expert_w": nrm(ks[22], (DEPTH, D_MODEL, N_EXPERTS), D_MODEL ** -0.5),
        "router_expert_b": nrm(ks[23], (DEPTH, N_EXPERTS), 0.01),
        "expert_w1": nrm(ks[24], (DEPTH, N_EXPERTS, D_MODEL, D_EXPERT), D_MODEL ** -0.5),
        "expert_w3": nrm(ks[25], (DEPTH, N_EXPERTS, D_MODEL, D_EXPERT), D_MODEL ** -0.5),
        "expert_w2": nrm(ks[26], (DEPTH, N_EXPERTS, D_EXPERT, D_MODEL), D_EXPERT ** -0.5),
        "norm_final_w": 1.0 + nrm(ks[27], (D_MODEL,), 0.01),
    }


def reference(x, meta_tokens, norm_mix_w, w_in, s5_a_re, s5_a_im, s5_log_dt, s5_b_re, s5_b_im, s5_c_re,
              s5_c_im, s5_d, s5_glu_w, s5_glu_b, hg_lb_gamma, hg_norm_w, w_branch_a, w_branch_b, w_out,
              norm_ffn_w, router_group_w, router_group_b, router_expert_w, router_expert_b,
              expert_w1, expert_w3, expert_w2, norm_final_w):
    B = x.shape[0]
    meta = jnp.broadcast_to(meta_tokens.astype(x.dtype)[None], (B, N_META, x.shape[-1]))
    h = jnp.concatenate([meta, x], axis=1)
    for l in range(DEPTH):
        a = rmsnorm(h, norm_mix_w[l])
        h = h + hybrid_mixer(a, l, w_in[l], s5_a_re[l], s5_a_im[l], s5_log_dt[l], s5_b_re[l], s5_b_im[l],
                             s5_c_re[l], s5_c_im[l], s5_d[l], s5_glu_w[l], s5_glu_b[l], hg_lb_gamma,
                             hg_norm_w[l], w_branch_a[l], w_branch_b[l], w_out[l])
        f = rmsnorm(h, norm_ffn_w[l])
        h = h + hierarchical_moe(f, router_group_w[l], router_group_b[l], router_expert_w[l], router_expert_b[l],
                                 expert_w1[l], expert_w3[l], expert_w2[l])
    h = rmsnorm(h, norm_final_w)
    return h[:, N_META:]
```

```python
import numpy as np
import concourse.bass as bass
import concourse.mybir as mybir
from concourse.bass_utils import run_bass_kernel_spmd

F32 = mybir.dt.float32
BF16 = mybir.dt.bfloat16
I32 = mybir.dt.int32
ALU = mybir.AluOpType
AF = mybir.ActivationFunctionType
AX = mybir.AxisListType

D = 4096
DH = 2048
NCORES = 8
TOWN = 2048
NPRE = 16


class T:
    def __init__(self, t, name):
        self.t = t
        self.name = name
        self.w = None
        self.r = {}

    def __getitem__(self, k):
        return self.t[k]


class Prog:
    ENGS = ["pe", "dve", "act", "pool", "sp"]

    def __init__(self, nc):
        self.nc = nc
        self.items = {e: [] for e in self.ENGS}
        self.seq = {e: 0 for e in self.ENGS}
        self.sem = {e: nc.alloc_semaphore("s_" + e) for e in self.ENGS}
        self.seen = {e: {} for e in self.ENGS}
        self.lanes = {}
        self.lane_rr = {}
        for q, n in (("sp", 8), ("act", 4), ("pool", 8)):
            self.lanes[q] = [[nc.alloc_semaphore(f"l_{q}{i}"), 0, (q, i)] for i in range(n)]
            self.lane_rr[q] = 0
        self.n = 0

    def sb(self, name, shape, dt):
        return T(self.nc.alloc_sbuf_tensor(name, list(shape), dt), name)

    def ps(self, name, shape, dt=F32):
        return T(self.nc.alloc_psum_tensor(name, list(shape), dt), name)

    def dram(self, name, shape, dt, kind="Internal"):
        return T(self.nc.dram_tensor(name, list(shape), dt, kind=kind).ap(), name)

    def _wait(self, eng, dep):
        kind, key, val = dep
        if kind == "eng":
            if key == eng and eng == "pe":
                return
            k = ("e", key)
            if self.seen[eng].get(k, 0) >= val:
                return
            self.seen[eng][k] = val
            self.items[eng].append(("wait", self.sem[key], val))
        else:
            q, i = key
            k = ("l", key)
            if self.seen[eng].get(k, 0) >= val:
                return
            self.seen[eng][k] = val
            self.items[eng].append(("wait", self.lanes[q][i][0], 16 * val))

    def _deps(self, eng, reads, writes):
        for r in reads:
            if r.w is not None:
                self._wait(eng, r.w)
        for w in writes:
            if w.w is not None:
                self._wait(eng, w.w)
            for k, v in w.r.items():
                self._wait(eng, (k[0], k[1], v))

    def _mark(self, tag, reads, writes):
        for r in reads:
            r.r[(tag[0], tag[1])] = tag[2]
        for w in writes:
            w.w = tag
            w.r = {}

    def op(self, eng, fn, reads=(), writes=()):
        self._deps(eng, reads, writes)
        self.seq[eng] += 1
        self.items[eng].append(("ins", fn, self.sem[eng], 1))
        self._mark(("eng", eng, self.seq[eng]), reads, writes)
        self.n += 1

    def dma(self, q, out_ap, in_ap, reads=(), writes=(), fn=None):
        lanes = self.lanes[q]
        li = self.lane_rr[q]
        self.lane_rr[q] = (li + 1) % len(lanes)
        lane = lanes[li]
        if lane[1] > 0:
            self._wait(q, ("lane", lane[2], lane[1]))
        self._deps(q, reads, writes)
        lane[1] += 1
        if fn is None:
            fn = lambda e, o=out_ap, i=in_ap: e.dma_start(out=o, in_=i)
        self.items[q].append(("ins", fn, lane[0], 16))
        self._mark(("lane", lane[2], lane[1]), reads, writes)
        self.n += 1

    def finish(self):
        for q in ("sp", "act", "pool"):
            for lane in self.lanes[q]:
                if lane[1] > 0:
                    self._wait("sp", ("lane", lane[2], lane[1]))
        for e in self.ENGS:
            if e != "sp" and self.seq[e] > 0:
                self._wait("sp", ("eng", e, self.seq[e]))

    def emit(self):
        nc = self.nc
        items = self.items

        def run(e, lst):
            for it in lst:
                if it[0] == "wait":
                    e.wait_ge(it[1], it[2])
                else:
                    it[1](e).then_inc(it[2], it[3])

        with nc.Block() as block:
            @block.tensor
            def _(e):
                run(e, items["pe"])

            @block.vector
            def _(e):
                run(e, items["dve"])

            @block.scalar
            def _(e):
                run(e, items["act"])

            @block.gpsimd
            def _(e):
                run(e, items["pool"])

            @block.sync
            def _(e):
                run(e, items["sp"])


def prod(xs):
    r = 1
    for v in xs:
        r *= v
    return r


class Arena:
    def __init__(self, P, nfloat):
        self.P = P
        self.t = P.nc.alloc_sbuf_tensor("arena", [128, nfloat], F32)
        self.n = nfloat
        self.off = 0
        self.k = 0
        self.pp = P.nc.alloc_psum_tensor("psum_all", [128, 8, 512], F32)

    def reset(self):
        self.P.barrier()
        self.off = 0

    def tile(self, shape, dt, name=None):
        free = prod(shape[1:])
        nfl = free if dt in (F32, I32) else (free + 1) // 2
        nfl = (nfl + 7) // 8 * 8
        assert self.off + nfl <= self.n, f"arena overflow {name} {self.off + nfl} > {self.n}"
        ap = self.t[:, self.off:self.off + nfl]
        self.off += nfl
        if dt != F32:
            ap = ap.bitcast(dt)
        ap = ap[:, 0:free]
        if len(shape) == 3:
            ap = ap.rearrange("p (a b) -> p a b", a=shape[1])
        elif len(shape) == 4:
            ap = ap.rearrange("p (a b c) -> p a b c", a=shape[1], b=shape[2])
        self.k += 1
        return T(ap, name or f"t{self.k}")

    def psum(self, bank, nbanks=1, dt=F32, shape=None, name=None):
        ap = self.pp[:, bank:bank + nbanks, :].rearrange("p a b -> p (a b)")
        if dt != F32:
            ap = ap.bitcast(dt)
        if shape is not None:
            free = prod(shape[1:])
            ap = ap[:, 0:free]
            if len(shape) == 3:
                ap = ap.rearrange("p (a b) -> p a b", a=shape[1])
        self.k += 1
        return T(ap, name or f"ps{self.k}")


def _barrier(self):
    for e in self.ENGS:
        for o in self.ENGS:
            if o != e and self.seq[o] > 0:
                self._wait(e, ("eng", o, self.seq[o]))
        for q in ("sp", "act", "pool"):
            for lane in self.lanes[q]:
                if lane[1] > 0:
                    self._wait(e, ("lane", lane[2], lane[1]))


Prog.barrier = _barrier


def L(name, *args, **kw):
    return lambda e: getattr(e, name)(*args, **kw)


NTOK = 2 * TOWN + 2 * NPRE
CAP = 256
NEXP = 64

FAMS = [("u", 0, 2048, "fm", "all"), ("q", 2048, 2048, "tm", "own"), ("v", 4096, 2048, "tm", "all"),
        ("f1", 6144, 2048, "tm", "ownpre"), ("f2", 8192, 2048, "tm", "all"), ("go", 10240, 2048, "tm", "own"),
        ("ga", 12288, 4096, "fm", "own"), ("gb", 16384, 4096, "fm", "own")]


def build(debug=False, stages=99):
    nc = bass.Bass("TRN2", target_bir_lowering=False)
    P = Prog(nc)
    A = Arena(P, 50176)
    okind = "ExternalOutput" if debug else "Internal"

    def din(name, shape, dt=F32):
        return T(nc.dram_tensor(name, list(shape), dt, kind="ExternalInput").ap(), name)

    x_own = din("x_own", [TOWN, D])
    x_for = din("x_for", [TOWN, D])
    x_pre = din("x_pre", [2 * NPRE, D])
    nmw = din("norm_mix_w", [1, D])
    w_in = din("w_in", [D, 20480])
    ident_d = din("ident", [128, 128])
    lbg = din("lbg", [2, 2, DH])
    out = T(nc.dram_tensor("out", [TOWN, D], F32, kind="ExternalOutput").ap(), "out")
    iota_d = din("iota", [128, 1024])
    s5A_d = din("s5A", [2, 3, 128, 32, 128])
    s5B_d = din("s5B", [2, 2, 128, 32, 128])
    s5S_d = din("s5S", [2, 3, 128, 64])
    s5C_d = din("s5C", [2, 2, 128, 64, 64])
    s5d_d = din("s5d", [128, 16])
    tri_d = din("tri", [4, 128, 128])
    mask_d = din("mask", [2, 64, 64])
    hgnw_d = din("hg_norm_w", [1, DH])
    gluw_d = din("s5_glu_w", [DH, DH])
    glub_d = din("glub", [128, 16])
    wba_d = din("w_branch_a", [DH, D])
    wbb_d = din("w_branch_b", [DH, D])
    wout_d = din("w_out", [D, D])
    nfw_d = din("norm_ffn_w", [1, D])
    rw_d = din("rw", [D, 72])
    rb_d = din("rb", [1, 72])
    gcap_d = din("gcap", [1, 8])
    tris_d = din("tris", [128, 128])
    w1_d = din("expert_w1", [NEXP, D, 512])
    w3_d = din("expert_w3", [NEXP, D, 512])
    w2_d = din("expert_w2", [NEXP, 512, D])
    nfin_d = din("norm_final_w", [1, D])

    aT = P.dram("aT", [D, NTOK], BF16, okind)
    uT = P.dram("uT", [DH, NTOK], F32, okind)
    q_s = P.dram("q_s", [TOWN, DH], F32, okind)
    v_s = P.dram("v_s", [NTOK, DH], F32, okind)
    f1_s = P.dram("f1_s", [NTOK, DH], F32, okind)
    f2_s = P.dram("f2_s", [NTOK, DH], F32, okind)
    go_s = P.dram("go_s", [TOWN, DH], F32, okind)
    ga_s = P.dram("ga_s", [D, TOWN], F32, okind)
    gb_s = P.dram("gb_s", [D, TOWN], F32, okind)
    y1T = P.dram("y1T", [DH, TOWN], F32, okind)
    zT = P.dram("zT", [DH, TOWN], BF16, okind)
    o_s = P.dram("o_s", [TOWN, DH], F32, okind)
    hgT = P.dram("hgT", [DH, TOWN], BF16, okind)
    s5oT = P.dram("s5oT", [DH, TOWN], BF16, okind)
    mA_s = P.dram("mA_s", [D, TOWN], F32, okind)
    mT = P.dram("mT", [D, TOWN], BF16, okind)
    hmid = P.dram("hmid", [TOWN, D], F32, okind)
    Xs = P.dram("Xs", [8 * 384, D + 16], BF16)
    Yd = P.dram("Yd", [8 * 384, D], F32)
    fam_out = {"u": uT, "q": q_s, "v": v_s, "f1": f1_s, "f2": f2_s, "go": go_s, "ga": ga_s, "gb": gb_s}

    ident = T(nc.alloc_sbuf_tensor("ident_sb", [128, 128], F32), "ident")
    identb = T(nc.alloc_sbuf_tensor("identb", [128, 128], BF16), "identb")
    P.dma("sp", ident[:, :], ident_d[:, :], writes=[ident])
    P.op("dve", L("tensor_copy", identb[:, :], ident[:, :]), reads=[ident], writes=[identb])

    nmwB = A.tile([128, D], F32, "nmwB")
    P.dma("sp", nmwB[:, :], nmw[0:1, :].partition_broadcast(128), writes=[nmwB])
    xt = [A.tile([128, D], F32, f"xt{i}") for i in range(2)]
    ab = [A.tile([128, D], BF16, f"ab{i}") for i in range(2)]
    ss = [A.tile([128, 8], F32, f"ss{i}") for i in range(2)]
    junk = A.tile([128, D], BF16, "junk")
    aTt = [A.tile([128, 32, 128], BF16, f"aTt{i}") for i in range(2)]
    pst = [A.psum(i, 1, BF16, [128, 4, 128], f"pst{i}") for i in range(2)]

    def rms_tile(X, S, nrow, wB, Aout):
        P.op("act", L("activation", junk[:nrow, :], X[:nrow, :], AF.Square, accum_out=S[:nrow, 0:1]),
             reads=[X], writes=[junk, S])
        P.op("dve", L("tensor_scalar", S[:nrow, 1:2], S[:nrow, 0:1], 1.0 / D, 1e-6, ALU.mult, ALU.add),
             reads=[S], writes=[S])
        P.op("act", L("activation", S[:nrow, 2:3], S[:nrow, 1:2], AF.Sqrt), reads=[S], writes=[S])
        P.op("dve", L("reciprocal", S[:nrow, 3:4], S[:nrow, 2:3]), reads=[S], writes=[S])
        P.op("dve", L("scalar_tensor_tensor", Aout[:nrow, :], X[:nrow, :], S[:nrow, 3:4], wB[:nrow, :],
                                                     ALU.mult, ALU.mult), reads=[X, S, wB], writes=[Aout])

    def transpose_to(Asrc, nrow, AT, idt, pstiles):
        for g in range(8):
            ps = pstiles[g % 2]
            for j in range(4):
                k = g * 4 + j
                P.op("pe", L("transpose", ps[:, j, :nrow], Asrc[:nrow, k * 128:(k + 1) * 128],
                                                                  idt[:nrow, :nrow]), reads=[Asrc, idt], writes=[ps])
            if g % 2 == 0:
                P.op("dve", L("tensor_copy", AT[:, g * 4:(g + 1) * 4, :nrow], ps[:, :, :nrow]),
                     reads=[ps], writes=[AT])
            else:
                P.op("act", L("copy", AT[:, g * 4:(g + 1) * 4, :nrow], ps[:, :, :nrow]),
                     reads=[ps], writes=[AT])

    def norm_tile(src, r0, nrow, col0, i):
        X, Ab, S, AT = xt[i % 2], ab[i % 2], ss[i % 2], aTt[i % 2]
        P.dma("sp", X[:nrow, :], src[r0:r0 + nrow, :], writes=[X])
        rms_tile(X, S, nrow, nmwB, Ab)
        transpose_to(Ab, nrow, AT, identb, pst)
        P.dma("act", aT[:, col0:col0 + nrow].rearrange("(k p) t -> p k t", p=128), AT[:, :, :nrow],
              reads=[AT], writes=[aT])

    import os as _os
    SKIP = _os.environ.get("SKIP123")
    i = 0
    for tt in range(0 if SKIP else TOWN // 128):
        norm_tile(x_own, tt * 128, 128, tt * 128, i); i += 1
    for tt in range(0 if SKIP else TOWN // 128):
        norm_tile(x_for, tt * 128, 128, TOWN + tt * 128, i); i += 1
    norm_tile(x_pre, 0, 2 * NPRE, 2 * TOWN, i); i += 1

    def gemm(XT, K, col0, ncol, W, blocks, wb, epi):
        KC = K // 128
        xg = A.tile([128, KC, ncol], BF16, "xg")
        for k4 in range(0, KC, 8):
            P.dma("sp", xg[:, k4:k4 + 8, :], XT[k4 * 128:(k4 + 8) * 128, col0:col0 + ncol].rearrange("(k p) t -> p k t", p=128),
                  reads=[XT], writes=[xg])
        wbuf = [A.tile([128, KC, wb], BF16, f"wbuf{i}") for i in range(2)]
        pss = [A.psum(b, 1, F32, None, f"gps{b}") for b in range(6)]
        pi = 0
        for bi, (wc0, mode, need) in enumerate(blocks):
            Wt = wbuf[bi % 2]
            P.dma("pool", Wt[:, :, :], W[:, wc0:wc0 + wb].rearrange("(k p) n -> p k n", p=128), reads=[W], writes=[Wt])
            if mode == "fm":
                for m in range(wb // 128):
                    for n0 in range(0, ncol, 512):
                        nn = min(512, ncol - n0)
                        if not need(n0):
                            continue
                        ps = pss[pi % 6]; pi += 1
                        for k in range(KC):
                            P.op("pe", L("matmul", ps[:, :nn], Wt[:, k, m * 128:(m + 1) * 128], xg[:, k, n0:n0 + nn],
                                start=(k == 0), stop=(k == KC - 1)), reads=[Wt, xg], writes=[ps])
                        epi(ps, "fm", wc0 + m * 128, 128, col0 + n0, nn, xg)
            else:
                for t0 in range(0, ncol, 128):
                    rows = min(128, ncol - t0)
                    if not need(t0):
                        continue
                    ps = pss[pi % 6]; pi += 1
                    for k in range(KC):
                        P.op("pe", L("matmul", ps[:rows, :wb], xg[:, k, t0:t0 + rows], Wt[:, k, :],
                            start=(k == 0), stop=(k == KC - 1)), reads=[Wt, xg], writes=[ps])
                    epi(ps, "tm", wc0, wb, col0 + t0, rows, xg)

    if stages >= 2 and not SKIP:
        A.reset()
        lbB = [A.tile([128, DH], F32, f"lbB{d}") for d in range(2)]
        mark0 = A.off
        tmpg = A.tile([128, DH], F32, "tmpg")
        for d in range(2):
            P.dma("sp", lbB[d][:, :], lbg[d, 0:1, :].partition_broadcast(128), writes=[lbB[d]])
            P.dma("sp", tmpg[:, :], lbg[d, 1:2, :].partition_broadcast(128), writes=[tmpg])
            P.op("dve", L("tensor_tensor", lbB[d][:, :], lbB[d][:, :], tmpg[:, :], ALU.subtract),
                 reads=[lbB[d], tmpg], writes=[lbB[d]])
            P.op("act", L("activation", lbB[d][:, :], lbB[d][:, :], AF.Sigmoid), reads=[lbB[d]], writes=[lbB[d]])
        P.barrier(); A.off = mark0
        stg = [A.tile([128, 512], F32, f"stg{i}") for i in range(4)]
        si = [0]

        def proj_epi(ps, mode, c0, nc_, t0, nt, xg):
            fam = [f for f in FAMS if f[1] <= c0 < f[1] + f[2]][0]
            name, fc0 = fam[0], fam[1]
            S = stg[si[0] % 4]; si[0] += 1
            dst = fam_out[name]
            if mode == "fm":
                r0 = c0 - fc0
                if name == "u":
                    P.op("dve", L("tensor_copy", S[:, :nt], ps[:, :nt]), reads=[ps], writes=[S])
                else:
                    P.op("act", L("activation", S[:, :nt], ps[:, :nt], AF.Sigmoid), reads=[ps], writes=[S])
                P.dma("act", dst[r0:r0 + 128, t0:t0 + nt], S[:, :nt], reads=[S], writes=[dst])
            else:
                cc = c0 - fc0
                if name in ("q", "go"):
                    P.op("act", L("activation", S[:nt, :nc_], ps[:nt, :nc_], AF.Silu), reads=[ps], writes=[S])
                elif name == "v":
                    P.op("dve", L("tensor_copy", S[:nt, :nc_], ps[:nt, :nc_]), reads=[ps], writes=[S])
                else:
                    d = 0 if name == "f1" else 1
                    P.op("act", L("activation", S[:nt, :nc_], ps[:nt, :nc_], AF.Sigmoid), reads=[ps], writes=[S])
                    S2 = stg[si[0] % 4]; si[0] += 1
                    P.op("dve", L("tensor_scalar", S2[:nt, :nc_], S[:nt, :nc_], -1.0, 1.0, ALU.mult, ALU.add),
                         reads=[S], writes=[S2])
                    P.op("dve", L("tensor_tensor", S2[:nt, :nc_], S2[:nt, :nc_], lbB[d][:nt, cc:cc + nc_], ALU.mult),
                         reads=[S2, lbB[d]], writes=[S2])
                    P.op("dve", L("tensor_tensor", S[:nt, :nc_], S[:nt, :nc_], S2[:nt, :nc_], ALU.add),
                         reads=[S, S2], writes=[S])
                P.dma("act", dst[t0:t0 + nt, cc:cc + nc_], S[:nt, :nc_], reads=[S], writes=[dst])

        WB = 256
        blocks_own, blocks_for = [], []
        for (name, fc0, fn, mode, scope) in FAMS:
            for b in range(fn // WB):
                blocks_own.append((fc0 + b * WB, mode, lambda t: True))
                if scope == "all":
                    blocks_for.append((fc0 + b * WB, mode, lambda t: True))
                elif scope == "ownpre":
                    blocks_for.append((fc0 + b * WB, mode, lambda t: t >= TOWN))
        mark = A.off
        gemm(aT, D, 0, TOWN, w_in, blocks_own, WB, proj_epi)
        P.barrier(); A.off = mark
        gemm(aT, D, TOWN, TOWN + 2 * NPRE, w_in, blocks_for, WB, proj_epi)


    if stages >= 3 and not SKIP:
        A.reset()
        TWO_PI = float(2 * np.pi)
        io = A.tile([128, 1024], F32, "io")
        P.dma("sp", io[:, :], iota_d[:, :], writes=[io])
        s5dt = A.tile([128, 16], F32, "s5dt")
        P.dma("sp", s5dt[:, :], s5d_d[:, :], writes=[s5dt])
        seg_defs = [
            [(4096, 16, 1.0, 1.0, -1), (0, 1024, 1.0, 17.0, 0), (1024, 1024, 1.0, 1041.0, 1)],
            [(4112, 16, -1.0, 16.0, -1), (3072, 1024, -1.0, 1040.0, -1), (2048, 1024, -1.0, 2064.0, -1),
             (1024, 1024, -1.0, 3088.0, 1), (0, 1024, -1.0, 4112.0, 0)],
        ]
        mark_d = A.off
        for d in range(2):
            P.barrier(); A.off = mark_d
            segs = seg_defs[d]
            WBr = A.tile([128, 32, 128], BF16, "WBr")
            WBi = A.tile([128, 32, 128], BF16, "WBi")
            Cwr = A.tile([128, 64, 64], BF16, "Cwr")
            Cwi = A.tile([128, 64, 64], BF16, "Cwi")
            rho_s = A.tile([128, 64], F32, "rho_s")
            kap_s = A.tile([128, 64], F32, "kap_s")
            skap = A.tile([128, len(segs), 64], F32, "skap")
            bkap = A.tile([128, len(segs), 64], F32, "bkap")
            bkap25 = A.tile([128, len(segs), 64], F32, "bkap25")
            mark_setup = A.off
            tl = {n: A.tile([128, 1024], F32, "su_" + n) for n in
                  ["ar", "ai", "ld", "dt", "rho", "kap", "kf", "sn", "cs", "t1", "t2", "zr", "zi", "br", "bi"]}
            ki = A.tile([128, 1024], I32, "su_ki")

            def ew(eng, fn, reads, writes):
                P.op(eng, fn, reads=[tl[r] if isinstance(r, str) else r for r in reads],
                     writes=[tl[w] if isinstance(w, str) else w for w in writes])

            def TT(o, a, b, op, eng="dve"):
                ew(eng, L("tensor_tensor", tl[o][:, :], tl[a][:, :], tl[b][:, :], op), [a, b], [o])

            def sincos(kapname):
                for outn, shift in (("sn", 0.0), ("cs", 0.25)):
                    ew("dve", L("tensor_scalar", tl["t1"][:, :], tl[kapname][:, :], 1.0, shift, ALU.mult, ALU.add),
                       [kapname], ["t1"])
                    ew("dve", L("tensor_copy", ki[:, :], tl["t1"][:, :]), ["t1"], [ki])
                    ew("dve", L("tensor_copy", tl["kf"][:, :], ki[:, :]), [ki], ["kf"])
                    TT("t1", "t1", "kf", ALU.subtract)
                    ew("act", L("activation", tl[outn][:, :], tl["t1"][:, :], AF.Sin, scale=TWO_PI), ["t1"], [outn])

            for qd in range(4):
                bsl = slice(8 * qd, 8 * qd + 8)
                for w_, nm in enumerate(["ar", "ai", "ld"]):
                    P.dma("sp", tl[nm][:, :].rearrange("p (a b) -> p a b", a=8), s5A_d[d, w_, :, bsl, :], writes=[tl[nm]])
                for w_, nm in enumerate(["br", "bi"]):
                    P.dma("sp", tl[nm][:, :].rearrange("p (a b) -> p a b", a=8), s5B_d[d, w_, :, bsl, :], writes=[tl[nm]])
                ew("act", L("activation", tl["dt"][:, :], tl["ld"][:, :], AF.Exp), ["ld"], ["dt"])
                TT("t1", "ar", "dt", ALU.mult)
                ew("act", L("activation", tl["rho"][:, :], tl["t1"][:, :], AF.Exp), ["t1"], ["rho"])
                TT("kap", "ai", "dt", ALU.mult)
                ew("dve", L("tensor_scalar", tl["kap"][:, :], tl["kap"][:, :], 1.0 / TWO_PI, None, ALU.mult), ["kap"], ["kap"])
                sincos("kap")
                TT("cs", "rho", "cs", ALU.mult)
                TT("sn", "rho", "sn", ALU.mult)
                TT("t1", "ar", "ar", ALU.mult)
                TT("t2", "ai", "ai", ALU.mult)
                TT("t1", "t1", "t2", ALU.add)
                ew("dve", L("reciprocal", tl["t1"][:, :], tl["t1"][:, :]), ["t1"], ["t1"])
                ew("dve", L("tensor_scalar", tl["cs"][:, :], tl["cs"][:, :], -1.0, None, ALU.add), ["cs"], ["cs"])
                TT("zr", "cs", "ar", ALU.mult)
                TT("t2", "sn", "ai", ALU.mult)
                TT("zr", "zr", "t2", ALU.add)
                TT("zr", "zr", "t1", ALU.mult)
                TT("zi", "sn", "ar", ALU.mult)
                TT("t2", "cs", "ai", ALU.mult)
                TT("zi", "zi", "t2", ALU.subtract)
                TT("zi", "zi", "t1", ALU.mult)
                TT("t1", "zr", "br", ALU.mult)
                TT("t2", "zi", "bi", ALU.mult)
                ew("dve", L("tensor_tensor", WBr[:, bsl, :], tl["t1"][:, :].rearrange("p (a b) -> p a b", a=8),
                                                            tl["t2"][:, :].rearrange("p (a b) -> p a b", a=8), ALU.subtract),
                   ["t1", "t2"], [WBr])
                TT("t1", "zr", "bi", ALU.mult)
                TT("t2", "zi", "br", ALU.mult)
                ew("dve", L("tensor_tensor", WBi[:, bsl, :], tl["t1"][:, :].rearrange("p (a b) -> p a b", a=8),
                                                            tl["t2"][:, :].rearrange("p (a b) -> p a b", a=8), ALU.add),
                   ["t1", "t2"], [WBi])
            sA = {n: A.tile([128, 64], F32, "ss_" + n) for n in ["ar", "ai", "ld", "t"]}
            for w_, nm in enumerate(["ar", "ai", "ld"]):
                P.dma("sp", sA[nm][:, :], s5S_d[d, w_, :, :], writes=[sA[nm]])
            P.op("act", L("activation", sA["ld"][:, :], sA["ld"][:, :], AF.Exp), reads=[sA["ld"]], writes=[sA["ld"]])
            P.op("dve", L("tensor_tensor", sA["t"][:, :], sA["ar"][:, :], sA["ld"][:, :], ALU.mult),
                 reads=[sA["ar"], sA["ld"]], writes=[sA["t"]])
            P.op("act", L("activation", rho_s[:, :], sA["t"][:, :], AF.Exp), reads=[sA["t"]], writes=[rho_s])
            P.op("dve", L("tensor_tensor", kap_s[:, :], sA["ai"][:, :], sA["ld"][:, :], ALU.mult),
                 reads=[sA["ai"], sA["ld"]], writes=[kap_s])
            P.op("dve", L("tensor_scalar", kap_s[:, :], kap_s[:, :], 1.0 / TWO_PI, None, ALU.mult),
                 reads=[kap_s], writes=[kap_s])
            for si_, sg in enumerate(segs):
                P.op("dve", L("tensor_scalar", skap[:, si_, :], kap_s[:, :], sg[2], None, ALU.mult),
                     reads=[kap_s], writes=[skap])
                P.op("dve", L("tensor_scalar", bkap[:, si_, :], kap_s[:, :], sg[3], None, ALU.mult),
                     reads=[kap_s], writes=[bkap])
                P.op("dve", L("tensor_scalar", bkap25[:, si_, :], kap_s[:, :], sg[3], 0.25, ALU.mult, ALU.add),
                     reads=[kap_s], writes=[bkap25])
            for ri, Cw in enumerate([Cwr, Cwi]):
                for hh in range(4):
                    ctmp = tl["ar"]
                    P.dma("sp", ctmp[:, :].rearrange("p (a b) -> p a b", a=16), s5C_d[d, ri, :, 16 * hh:16 * hh + 16, :], writes=[ctmp])
                    P.op("dve", L("tensor_scalar", Cw[:, 16 * hh:16 * hh + 16, :], ctmp[:, :].rearrange("p (a b) -> p a b", a=16),
                        (1.0 if ri == 0 else -1.0), None, ALU.mult), reads=[ctmp], writes=[Cw])
            P.barrier(); A.off = mark_setup
            uf = A.tile([128, NTOK], F32, "uf")
            ub = A.tile([128, NTOK], BF16, "ub")
            csn = [[A.tile([128, 1024], F32, f"cs{i}"), A.tile([128, 1024], F32, f"sn{i}")] for i in range(2)]
            rr = A.tile([128, 1024], F32, "rr")
            rq = A.tile([128, 1024], F32, "rq")
            rf = A.tile([128, 1024], F32, "rf")
            ri_t = A.tile([128, 1024], I32, "ri")
            ri_t2 = A.tile([128, 1024], I32, "ri2")
            rf2 = A.tile([128, 1024], F32, "rf2")
            tt_ = [A.tile([128, 1024], F32, f"mt{i}") for i in range(4)]
            t5 = A.tile([128, 1024], F32, "mt5")
            t6 = A.tile([128, 1024], F32, "mt6")
            mm = A.tile([128, 2, 1024], F32, "mm")
            ww = A.tile([128, 2, 1024], F32, "ww")
            xr = A.tile([128, 1024], BF16, "xr")
            xi = A.tile([128, 1024], BF16, "xi")
            cr = A.tile([128, 2], F32, "cr")
            ys = [A.tile([128, 1024], F32, f"ys{i}") for i in range(2)]
            y1l = A.tile([128, 1024], F32, "y1l")
            zt = A.tile([128, 1024], BF16, "zt")
            bu_re = A.psum(0, 2, F32, None, "bu_re")
            bu_im = A.psum(2, 2, F32, None, "bu_im")
            psY = [A.psum(4, 2, F32, None, "psY0"), A.psum(6, 2, F32, None, "psY1")]
            tb = 0
            for c in range(16):
                P.dma("sp", uf[:, :], uT[c * 128:(c + 1) * 128, :], reads=[uT], writes=[uf])
                P.op("act", L("copy", ub[:, :], uf[:, :]), reads=[uf], writes=[ub])
                for hf in range(2):
                    hs = slice(hf * 64, hf * 64 + 64)
                    for pr in range(2):
                        un = c * 4 + hf * 2 + pr
                        blk = c * 2 + pr
                        first = True
                        for si_, (col0, n, sign, base, oseg) in enumerate(segs):
                            CS, SN = csn[tb % 2]; tb += 1
                            for TAB, src, ri_x, rf_x, bk in ((SN, rr, ri_t, rf, bkap), (CS, rq, ri_t2, rf2, bkap25)):
                                P.op("act", L("activation", src[:, :n], io[:, :n], AF.Identity, bias=bk[:, si_, un:un + 1], scale=skap[:, si_, un:un + 1]),
                                     reads=[io, skap, bk], writes=[src])
                                P.op("dve", L("tensor_copy", ri_x[:, :n], src[:, :n]), reads=[src], writes=[ri_x])
                                P.op("dve", L("tensor_copy", rf_x[:, :n], ri_x[:, :n]), reads=[ri_x], writes=[rf_x])
                                P.op("dve", L("tensor_tensor", rf_x[:, :n], src[:, :n], rf_x[:, :n], ALU.subtract),
                                     reads=[src, rf_x], writes=[rf_x])
                                P.op("act", L("activation", TAB[:, :n], rf_x[:, :n], AF.Sin, scale=TWO_PI),
                                     reads=[rf_x], writes=[TAB])
                            for n0 in range(0, n, 512):
                                nn = min(512, n - n0)
                                for PSB, WBx in ((bu_re, WBr), (bu_im, WBi)):
                                    P.op("pe", L("matmul", PSB[:, n0:n0 + nn], WBx[hs, blk, :], ub[hs, col0 + n0:col0 + n0 + nn], start=True, stop=True),
                                        reads=[WBx, ub], writes=[PSB])
                            t1, t2, t3, t4 = tt_
                            P.op("dve", L("tensor_tensor", t1[:, :n], bu_re[:, :n], CS[:, :n], ALU.mult), reads=[bu_re, CS], writes=[t1])
                            P.op("dve", L("tensor_tensor", t2[:, :n], bu_im[:, :n], SN[:, :n], ALU.mult), reads=[bu_im, SN], writes=[t2])
                            P.op("dve", L("tensor_tensor", mm[:, 0, :n], t1[:, :n], t2[:, :n], ALU.add), reads=[t1, t2], writes=[mm])
                            P.op("dve", L("tensor_tensor", t3[:, :n], bu_im[:, :n], CS[:, :n], ALU.mult), reads=[bu_im, CS], writes=[t3])
                            P.op("dve", L("tensor_tensor", t4[:, :n], bu_re[:, :n], SN[:, :n], ALU.mult), reads=[bu_re, SN], writes=[t4])
                            P.op("dve", L("tensor_tensor", mm[:, 1, :n], t3[:, :n], t4[:, :n], ALU.subtract), reads=[t3, t4], writes=[mm])
                            for j in range(2):
                                init = 0.0 if first else cr[:, j:j + 1]
                                rb = rho_s[:, un:un + 1].to_broadcast([128, n])
                                if d == 0:
                                    P.op("dve", L("tensor_tensor_scan", ww[:, j, :n], rb, mm[:, j, :n], init, ALU.mult, ALU.add), reads=[mm, rho_s, cr], writes=[ww])
                                else:
                                    P.op("dve", L("tensor_tensor_scan", ww[:, j, 0:n][:, ::-1], rb, mm[:, j, 0:n][:, ::-1], init, ALU.mult, ALU.add),
                                        reads=[mm, rho_s, cr], writes=[ww])
                            last = n - 1 if d == 0 else 0
                            P.op("dve", L("tensor_copy", cr[:, :], ww[:, :, last]), reads=[ww], writes=[cr])
                            first = False
                            if oseg >= 0:
                                P.op("pool", L("tensor_tensor", t5[:, :n], ww[:, 0, :n], CS[:, :n], ALU.mult), reads=[ww, CS], writes=[t5])
                                P.op("pool", L("tensor_tensor", t6[:, :n], ww[:, 1, :n], SN[:, :n], ALU.mult), reads=[ww, SN], writes=[t6])
                                P.op("pool", L("tensor_tensor", xr[:, :n], t5[:, :n], t6[:, :n], ALU.subtract), reads=[t5, t6], writes=[xr])
                                P.op("dve", L("tensor_tensor", t3[:, :n], ww[:, 0, :n], SN[:, :n], ALU.mult), reads=[ww, SN], writes=[t3])
                                P.op("dve", L("tensor_tensor", t4[:, :n], ww[:, 1, :n], CS[:, :n], ALU.mult), reads=[ww, CS], writes=[t4])
                                P.op("dve", L("tensor_tensor", xi[:, :n], t3[:, :n], t4[:, :n], ALU.add), reads=[t3, t4], writes=[xi])
                                PY = psY[oseg]
                                for n0 in range(0, n, 512):
                                    P.op("pe", L("matmul", PY[hs, n0:n0 + 512], Cwr[:, un, :], xr[:, n0:n0 + 512], start=(pr == 0), stop=False),
                                        reads=[Cwr, xr], writes=[PY])
                                    P.op("pe", L("matmul", PY[hs, n0:n0 + 512], Cwi[:, un, :], xi[:, n0:n0 + 512], start=False, stop=(pr == 1)),
                                        reads=[Cwi, xi], writes=[PY])
                for oseg in range(2):
                    cols = slice(oseg * 1024, oseg * 1024 + 1024)
                    YS = ys[oseg]
                    if d == 0:
                        P.op("dve", L("scalar_tensor_tensor", YS[:, :], uf[:, cols], s5dt[:, c:c + 1], psY[oseg][:, :], ALU.mult, ALU.add),
                            reads=[uf, s5dt, psY[oseg]], writes=[YS])
                        P.dma("act", y1T[c * 128:(c + 1) * 128, cols], YS[:, :], reads=[YS], writes=[y1T])
                    else:
                        P.dma("sp", y1l[:, :], y1T[c * 128:(c + 1) * 128, cols], reads=[y1T], writes=[y1l])
                        P.op("dve", L("tensor_tensor", YS[:, :], psY[oseg][:, :], y1l[:, :], ALU.add),
                             reads=[psY[oseg], y1l], writes=[YS])
                        P.op("act", L("activation", zt[:, :], YS[:, :], AF.Gelu), reads=[YS], writes=[zt])
                        P.dma("act", zT[c * 128:(c + 1) * 128, cols], zt[:, :], reads=[zt], writes=[zT])

    if stages >= 4:
        A.reset()
        tri = A.tile([128, 4, 128], F32, "tri")
        P.dma("sp", tri[:, :, :], tri_d[:, :, :].rearrange("a p t -> p a t"), writes=[tri])
        msk = A.tile([64, 2, 64], F32, "msk")
        P.dma("sp", msk[:64, :, :], mask_d[:, :, :].rearrange("a p t -> p a t"), writes=[msk])
        ones = A.tile([128, 1], F32, "ones")
        P.op("dve", L("memset", ones[:, :], 1.0), writes=[ones])
        nwB = A.tile([64, DH], F32, "nwB")
        P.dma("sp", nwB[:64, :], hgnw_d[0:1, :].partition_broadcast(64), writes=[nwB])
        S = A.tile([128, 16, 128], F32, "S")
        Sb = A.tile([128, 16, 128], BF16, "Sb")
        ft = A.tile([128, DH], F32, "ft"); vt = A.tile([128, DH], F32, "vt"); qt = A.tile([128, DH], F32, "qt")
        gt = A.tile([128, DH], F32, "gt"); kt = A.tile([128, DH], F32, "kt")
        vb = A.tile([128, DH], BF16, "vb")
        v2 = A.tile([64, 2, DH], F32, "v2"); vb2 = A.tile([64, 2, DH], BF16, "vb2")
        esuf = A.tile([128, 512], F32, "esuf"); eb = A.tile([128, 512], F32, "eb"); enb = A.tile([128, 512], F32, "enb")
        kh = A.tile([128, 512], BF16, "kh"); qtl = A.tile([128, 512], BF16, "qtl"); ktl = A.tile([128, 512], BF16, "ktl")
        qkT = A.tile([128, 16, 64], BF16, "qkT")
        qkT8 = T(qkT[:, :, :].rearrange("p (a b) c -> p a (b c)", b=2), "qkT8v")
        sT = A.tile([64, 8, 64], BF16, "sT")
        ebt = A.tile([128, 4], F32, "ebt")
        o_sb = A.tile([64, 2, DH], F32, "o_sb")
        o1l = A.tile([64, 2, DH], F32, "o1l")
        gol = A.tile([64, 2, DH], F32, "gol")
        hgb = A.tile([64, 2, DH], BF16, "hgb")
        hT = A.tile([128, 16, 128], BF16, "hT")
        rs = A.tile([64, 4, 32], F32, "rs")
        ps_suf = A.psum(0, 1, F32, None, "ps_suf")
        ps_b = A.psum(1, 1, F32, None, "ps_b")
        ps_T = A.psum(2, 1, BF16, [128, 16, 64], "ps_T")
        ps_T8 = T(ps_T[:, :, :].rearrange("p (a b) c -> p a (b c)", b=2), "ps_T8v")
        ps_sc = A.psum(3, 1, F32, [128, 8, 64], "ps_sc")
        ps_o = A.psum(4, 2, F32, [128, 2, 512], "ps_o")
        ps_dS = A.psum(6, 1, F32, [128, 4, 128], "ps_dS")
        ps_bt = A.psum(7, 1, F32, None, "ps_bt")
        for d in range(2):
            fsrc = f1_s if d == 0 else f2_s
            P.op("dve", L("memset", S[:, :, :], 0.0), writes=[S])
            P.op("dve", L("memset", Sb[:, :, :], 0.0), writes=[Sb])
            if d == 0:
                tiles = [(4096, 16, False)] + [(128 * t_, 128, True) for t_ in range(16)]
            else:
                tiles = [(4112, 16, False)] + [(128 * t_, 128, False) for t_ in range(31, 15, -1)] + \
                        [(128 * t_, 128, True) for t_ in range(15, -1, -1)]
            import os as _os
            if _os.environ.get("HG_LIMIT"):
                lim = _os.environ["HG_LIMIT"].split(",")
                if d > int(lim[0]):
                    continue
                tiles = tiles[int(lim[1]):int(lim[2])]
            for (row0, nrows, is_out) in tiles:
                nch = (nrows + 63) // 64
                P.dma("sp", ft[:nrows, :], fsrc[row0:row0 + nrows, :], reads=[fsrc], writes=[ft])
                P.dma("sp", vt[:nrows, :], v_s[row0:row0 + nrows, :], reads=[v_s], writes=[vt])
                P.op("act", L("activation", gt[:nrows, :], ft[:nrows, :], AF.Ln), reads=[ft], writes=[gt])
                P.op("dve", L("tensor_scalar", kt[:nrows, :], ft[:nrows, :], -1.0, 1.0, ALU.mult, ALU.add), reads=[ft], writes=[kt])
                P.op("act", L("copy", vb[:nrows, :], vt[:nrows, :]), reads=[vt], writes=[vb])
                if is_out:
                    P.dma("sp", qt[:, :], q_s[row0:row0 + 128, :], reads=[q_s], writes=[qt])
                    P.dma("sp", v2[:64, :, :], v_s[row0:row0 + 128, :].rearrange("(c p) n -> p c n", p=64), reads=[v_s], writes=[v2])
                    P.op("act", L("copy", vb2[:64, :, :], v2[:64, :, :]), reads=[v2], writes=[vb2])
                for hg in range(4):
                    cs_ = slice(hg * 512, hg * 512 + 512)
                    P.op("pe", L("matmul", ps_suf[:nrows, :], tri[:nrows, 2 * d + 1, :nrows], gt[:nrows, cs_], start=True, stop=True),
                         reads=[tri, gt], writes=[ps_suf])
                    P.op("act", L("activation", esuf[:nrows, :], ps_suf[:nrows, :], AF.Exp), reads=[ps_suf], writes=[esuf])
                    P.op("dve", L("tensor_tensor", kh[:nrows, :], kt[:nrows, cs_], esuf[:nrows, :], ALU.mult), reads=[kt, esuf], writes=[kh])
                    if is_out:
                        P.op("pe", L("matmul", ps_b[:, :], tri[:, 2 * d, :], gt[:, cs_], start=True, stop=True), reads=[tri, gt], writes=[ps_b])
                        P.op("act", L("activation", eb[:, :], ps_b[:, :], AF.Exp), reads=[ps_b], writes=[eb])
                        P.op("act", L("activation", enb[:, :], ps_b[:, :], AF.Exp, scale=-1.0), reads=[ps_b], writes=[enb])
                        P.op("dve", L("tensor_tensor", qtl[:, :], qt[:, cs_], eb[:, :], ALU.mult), reads=[qt, eb], writes=[qtl])
                        P.op("dve", L("tensor_tensor", ktl[:, :], kt[:, cs_], enb[:, :], ALU.mult), reads=[kt, enb], writes=[ktl])
                        for which, src in enumerate([qtl, ktl]):
                            for hh in range(4):
                                P.op("pe", L("transpose", ps_T8[:, which * 4 + hh, :], src[:, hh * 128:(hh + 1) * 128], identb[:, :]),
                                     reads=[src, identb], writes=[ps_T])
                        P.op("act", L("copy", qkT8[:, :, :], ps_T8[:, :, :]), reads=[ps_T], writes=[qkT])
                        for idx in range(8):
                            cc_, hh_ = idx // 4, idx % 4
                            P.op("pe", L("matmul", ps_sc[:64, idx, :], qkT8[:, 4 + hh_, cc_ * 64:cc_ * 64 + 64], qkT8[:, hh_, cc_ * 64:cc_ * 64 + 64],
                                         start=True, stop=True), reads=[qkT], writes=[ps_sc])
                        P.op("dve", L("tensor_tensor", sT[:64, :, :], ps_sc[:64, :, :],
                                      msk[:64, d:d + 1, :].to_broadcast([64, 8, 64]), ALU.mult), reads=[ps_sc, msk], writes=[sT])
                    order = list(range(nch)) if d == 0 else list(range(nch - 1, -1, -1))
                    for cc in order:
                        r0 = 64 * cc
                        nr = min(64, nrows - r0)
                        for hh in range(4):
                            head = hg * 4 + hh
                            idx = cc * 4 + hh
                            hc = slice(head * 128, head * 128 + 128)
                            if is_out:
                                P.op("pe", L("matmul", ps_o[:64, cc, hh * 128:(hh + 1) * 128], qkT8[:, hh, cc * 64:cc * 64 + 64], Sb[:, head, :], start=True, stop=False),
                                     reads=[qkT, Sb], writes=[ps_o])
                                P.op("pe", L("matmul", ps_o[:64, cc, hh * 128:(hh + 1) * 128], sT[:64, idx, :], vb2[:64, cc, hc], start=False, stop=True),
                                     reads=[sT, vb2], writes=[ps_o])
                            P.op("pe", L("matmul", ps_dS[:, hh, :], kh[r0:r0 + nr, hh * 128:(hh + 1) * 128], vb[r0:r0 + nr, hc], start=True, stop=True),
                                 reads=[kh, vb], writes=[ps_dS])
                            P.op("pe", L("matmul", ps_bt[:, hh:hh + 1], gt[r0:r0 + nr, hc], ones[r0:r0 + nr, 0:1], start=True, stop=True),
                                 reads=[gt, ones], writes=[ps_bt])
                        P.op("act", L("activation", ebt[:, :], ps_bt[:, 0:4], AF.Exp), reads=[ps_bt], writes=[ebt])
                        for hh in range(4):
                            head = hg * 4 + hh
                            P.op("dve", L("scalar_tensor_tensor", S[:, head, :], S[:, head, :], ebt[:, hh:hh + 1], ps_dS[:, hh, :], ALU.mult, ALU.add),
                                 reads=[S, ebt, ps_dS], writes=[S])
                        P.op("act", L("copy", Sb[:, hg * 4:hg * 4 + 4, :], S[:, hg * 4:hg * 4 + 4, :]), reads=[S], writes=[Sb])
                    if is_out:
                        P.op("act", L("copy", o_sb[:64, :, cs_], ps_o[:64, :, :]), reads=[ps_o], writes=[o_sb])
                if not is_out:
                    continue
                o_dst = o_s[row0:row0 + 128, :].rearrange("(c p) n -> p c n", p=64)
                if d == 0:
                    P.dma("act", o_dst, o_sb[:64, :, :], reads=[o_sb], writes=[o_s])
                    continue
                P.dma("sp", o1l[:64, :, :], o_dst, reads=[o_s], writes=[o1l])
                P.dma("sp", gol[:64, :, :], go_s[row0:row0 + 128, :].rearrange("(c p) n -> p c n", p=64), reads=[go_s], writes=[gol])
                P.op("dve", L("tensor_tensor", o_sb[:64, :, :], o_sb[:64, :, :], o1l[:64, :, :], ALU.add), reads=[o_sb, o1l], writes=[o_sb])
                P.op("pool", L("tensor_tensor", o1l[:64, :, :], o_sb[:64, :, :], o_sb[:64, :, :], ALU.mult), reads=[o_sb], writes=[o1l])
                P.op("dve", L("tensor_reduce", rs[:64, 0, :], o1l[:64, :, :].rearrange("p c (h v) -> p (c h) v", v=128), AX.X, ALU.add),
                     reads=[o1l], writes=[rs])
                P.op("dve", L("tensor_scalar", rs[:64, 1, :], rs[:64, 0, :], 1.0 / 128, 1e-6, ALU.mult, ALU.add), reads=[rs], writes=[rs])
                P.op("act", L("activation", rs[:64, 2, :], rs[:64, 1, :], AF.Sqrt), reads=[rs], writes=[rs])
                P.op("dve", L("reciprocal", rs[:64, 3, :], rs[:64, 2, :]), reads=[rs], writes=[rs])
                P.op("dve", L("tensor_tensor", o_sb[:64, :, :].rearrange("p c (h v) -> p (c h) v", v=128),
                              o_sb[:64, :, :].rearrange("p c (h v) -> p (c h) v", v=128),
                              rs[:64, 3, :].unsqueeze(2).to_broadcast([64, 32, 128]), ALU.mult), reads=[o_sb, rs], writes=[o_sb])
                P.op("pool", L("tensor_tensor", o_sb[:64, :, :], o_sb[:64, :, :], nwB[:64, :].unsqueeze(1).to_broadcast([64, 2, DH]), ALU.mult),
                     reads=[o_sb, nwB], writes=[o_sb])
                P.op("dve", L("tensor_tensor", hgb[:64, :, :], o_sb[:64, :, :], gol[:64, :, :], ALU.mult), reads=[o_sb, gol], writes=[hgb])
                for cc in range(2):
                    for c16 in range(16):
                        P.op("pe", L("transpose", ps_T[:, c16, :], hgb[:64, cc, c16 * 128:(c16 + 1) * 128], identb[:64, :64]),
                             reads=[hgb, identb], writes=[ps_T])
                    P.op("act", L("copy", hT[:, :, cc * 64:(cc + 1) * 64], ps_T[:, :, :]), reads=[ps_T], writes=[hT])
                P.dma("act", hgT[:, row0:row0 + 128].rearrange("(c p) t -> p c t", p=128), hT[:, :, :], reads=[hT], writes=[hgT])

    if stages >= 5:
        A.reset()
        glub = A.tile([128, 16], F32, "glub")
        P.dma("sp", glub[:, :], glub_d[:, :], writes=[glub])
        stg5 = [A.tile([128, 512], F32, f"s5g{i}") for i in range(4)]
        stb5 = [A.tile([128, 512], BF16, f"s5b{i}") for i in range(4)]
        lg5 = [A.tile([128, 512], F32, f"s5l{i}") for i in range(4)]
        k5 = [0]

        def glu_epi(ps, mode, c0, nc_, t0, nt, xg):
            i = k5[0] % 4; k5[0] += 1
            m = c0 // 128
            P.op("act", L("activation", stg5[i][:, :nt], ps[:, :nt], AF.Sigmoid, bias=glub[:, m:m + 1]), reads=[ps, glub], writes=[stg5[i]])
            P.op("dve", L("tensor_tensor", stb5[i][:, :nt], stg5[i][:, :nt], xg[:, m, t0:t0 + nt], ALU.mult), reads=[stg5[i], xg], writes=[stb5[i]])
            P.dma("act", s5oT[c0:c0 + 128, t0:t0 + nt], stb5[i][:, :nt], reads=[stb5[i]], writes=[s5oT])

        gemm(zT, DH, 0, TOWN, gluw_d, [(b * 512, "fm", lambda t: True) for b in range(4)], 512, glu_epi)

        A.reset()
        stg5 = [A.tile([128, 512], F32, f"s5g{i}") for i in range(4)]
        stb5 = [A.tile([128, 512], BF16, f"s5b{i}") for i in range(4)]
        lg5 = [A.tile([128, 512], F32, f"s5l{i}") for i in range(4)]
        lh5 = [A.tile([128, 512], F32, f"s5h{i}") for i in range(4)]
        stage5_mark = [A.off]

        def bra_epi(ps, mode, c0, nc_, t0, nt, xg):
            i = k5[0] % 4; k5[0] += 1
            P.dma("sp", lg5[i][:, :nt], ga_s[c0:c0 + 128, t0:t0 + nt], reads=[ga_s], writes=[lg5[i]])
            P.op("dve", L("tensor_tensor", stg5[i][:, :nt], ps[:, :nt], lg5[i][:, :nt], ALU.mult), reads=[ps, lg5[i]], writes=[stg5[i]])
            P.dma("act", mA_s[c0:c0 + 128, t0:t0 + nt], stg5[i][:, :nt], reads=[stg5[i]], writes=[mA_s])

        gemm(s5oT, DH, 0, TOWN, wba_d, [(b * 512, "fm", lambda t: True) for b in range(8)], 512, bra_epi)

        P.barrier()

        def brb_epi(ps, mode, c0, nc_, t0, nt, xg):
            i = k5[0] % 4; k5[0] += 1
            P.dma("sp", lg5[i][:, :nt], gb_s[c0:c0 + 128, t0:t0 + nt], reads=[gb_s], writes=[lg5[i]])
            P.dma("sp", lh5[i][:, :nt], mA_s[c0:c0 + 128, t0:t0 + nt], reads=[mA_s], writes=[lh5[i]])
            P.op("dve", L("tensor_tensor", stg5[i][:, :nt], ps[:, :nt], lg5[i][:, :nt], ALU.mult), reads=[ps, lg5[i]], writes=[stg5[i]])
            P.op("dve", L("tensor_tensor", stb5[i][:, :nt], stg5[i][:, :nt], lh5[i][:, :nt], ALU.add), reads=[stg5[i], lh5[i]], writes=[stb5[i]])
            P.dma("act", mT[c0:c0 + 128, t0:t0 + nt], stb5[i][:, :nt], reads=[stb5[i]], writes=[mT])

        A.off = stage5_mark[0]
        gemm(hgT, DH, 0, TOWN, wbb_d, [(b * 512, "fm", lambda t: True) for b in range(8)], 512, brb_epi)

        A.reset()
        stg5 = [A.tile([128, 512], F32, f"s5g{i}") for i in range(4)]
        lg5 = [A.tile([128, 512], F32, f"s5l{i}") for i in range(4)]

        def wout_epi(ps, mode, c0, nc_, t0, nt, xg):
            i = k5[0] % 4; k5[0] += 1
            P.dma("sp", lg5[i][:nt, :nc_], x_own[t0:t0 + nt, c0:c0 + nc_], reads=[x_own], writes=[lg5[i]])
            P.op("dve", L("tensor_tensor", stg5[i][:nt, :nc_], ps[:nt, :nc_], lg5[i][:nt, :nc_], ALU.add), reads=[ps, lg5[i]], writes=[stg5[i]])
            P.dma("act", hmid[t0:t0 + nt, c0:c0 + nc_], stg5[i][:nt, :nc_], reads=[stg5[i]], writes=[hmid])

        gemm(mT, D, 0, TOWN, wout_d, [(b * 256, "tm", lambda t: True) for b in range(16)], 256, wout_epi)

    if stages >= 6:
        GCAP = 384
        NGB = GCAP // 128
        XW = D + 16
        BIGI = float(8 * GCAP + 1000)
        destAll = T(nc.alloc_sbuf_tensor("destAll", [128, 16], I32), "destAll")
        A.reset()
        nfwB = A.tile([128, D], F32, "nfwB")
        P.dma("sp", nfwB[:, :], nfw_d[0:1, :].partition_broadcast(128), writes=[nfwB])
        RWt = A.tile([128, 32, 72], F32, "RWt")
        P.dma("sp", RWt[:, :, :], rw_d[:, :].rearrange("(k p) n -> p k n", p=128), writes=[RWt])
        rbB = A.tile([128, 72], F32, "rbB")
        P.dma("sp", rbB[:, :], rb_d[0:1, :].partition_broadcast(128), writes=[rbB])
        gcapB = A.tile([128, 8], F32, "gcapB")
        P.dma("sp", gcapB[:, :], gcap_d[0:1, :].partition_broadcast(128), writes=[gcapB])
        tris = A.tile([128, 128], F32, "tris")
        P.dma("sp", tris[:, :], tris_d[:, :], writes=[tris])
        ones2 = A.tile([128, 128], F32, "ones2")
        P.op("dve", L("memset", ones2[:, :], 1.0), writes=[ones2])
        cntB = A.tile([128, 8], F32, "cntB")
        P.op("dve", L("memset", cntB[:, :], 0.0), writes=[cntB])
        hx = [A.tile([128, D], F32, f"hx{i}") for i in range(2)]
        Ff = A.tile([128, D], F32, "Ff")
        Fb = [A.tile([128, XW], BF16, f"Fb{i}") for i in range(2)]
        FT = A.tile([128, 32, 128], F32, "FT")
        junk6 = A.tile([128, D], BF16, "junk6")
        ss6 = A.tile([128, 8], F32, "ss6")
        sm = A.tile([128, 24, 64], F32, "sm")
        pstf = [A.psum(i, 1, F32, [128, 4, 128], f"pstf{i}") for i in range(2)]
        ps_lg = A.psum(2, 1, F32, None, "ps_lg")
        ps_rk = A.psum(3, 1, F32, None, "ps_rk")
        ps_cs = A.psum(4, 1, F32, None, "ps_cs")

        def rms6(X, wB, Aout, junk_, S_):
            P.op("act", L("activation", junk_[:, :], X[:, :], AF.Square, accum_out=S_[:, 0:1]), reads=[X], writes=[junk_, S_])
            P.op("dve", L("tensor_scalar", S_[:, 1:2], S_[:, 0:1], 1.0 / D, 1e-6, ALU.mult, ALU.add), reads=[S_], writes=[S_])
            P.op("act", L("activation", S_[:, 2:3], S_[:, 1:2], AF.Sqrt), reads=[S_], writes=[S_])
            P.op("dve", L("reciprocal", S_[:, 3:4], S_[:, 2:3]), reads=[S_], writes=[S_])
            P.op("dve", L("scalar_tensor_tensor", Aout[:, :], X[:, :], S_[:, 3:4], wB[:, :], ALU.mult, ALU.mult), reads=[X, S_, wB], writes=[Aout])

        def sop(eng, name, *args, **kw):
            P.op(eng, L(name, *args, **kw), reads=[sm, ps_lg, ps_rk, ps_cs, rbB, gcapB, cntB], writes=[sm])

        LG, GMAX, NGMAX, OHG, EG, GSUM, GPROB, SEL, LIN, M1, OH1, LIN2, M2, OH2, DM, E2, DEN, RDEN, W1G, W2G, GW, RK, VAL, DST = range(24)
        for tt in range(16):
            X = hx[tt % 2]; FB = Fb[tt % 2]
            P.dma("sp", X[:, :], hmid[tt * 128:(tt + 1) * 128, :], reads=[hmid], writes=[X])
            rms6(X, nfwB, Ff, junk6, ss6)
            P.op("pool", L("tensor_copy", FB[:, 0:D], Ff[:, :]), reads=[Ff], writes=[FB])
            transpose_to(Ff, 128, FT, ident, pstf)
            for k in range(32):
                P.op("pe", L("matmul", ps_lg[:, 0:72], FT[:, k, :], RWt[:, k, :], start=(k == 0), stop=(k == 31)), reads=[FT, RWt], writes=[ps_lg])
            sop("dve", "tensor_tensor", sm[:, LG, 0:64], ps_lg[:, 8:72], rbB[:, 8:72], ALU.add)
            sop("dve", "tensor_tensor", sm[:, EG, 0:8], ps_lg[:, 0:8], rbB[:, 0:8], ALU.add)
            sop("dve", "tensor_reduce", sm[:, GMAX, 0:1], sm[:, EG, 0:8], AX.X, ALU.max)
            sop("dve", "tensor_scalar", sm[:, OHG, 0:8], sm[:, EG, 0:8], sm[:, GMAX, 0:1], None, ALU.is_equal)
            sop("dve", "tensor_scalar", sm[:, NGMAX, 0:1], sm[:, GMAX, 0:1], -1.0, None, ALU.mult)
            sop("act", "activation", sm[:, EG, 8:16], sm[:, EG, 0:8], AF.Exp, bias=sm[:, NGMAX, 0:1], accum_out=sm[:, GSUM, 0:1])
            sop("dve", "reciprocal", sm[:, GPROB, 0:1], sm[:, GSUM, 0:1])
            sop("dve", "tensor_tensor", sm[:, SEL, 0:64].rearrange("p (g j) -> p g j", g=8), sm[:, LG, 0:64].rearrange("p (g j) -> p g j", g=8),
                sm[:, OHG, 0:8].unsqueeze(2).to_broadcast([128, 8, 8]), ALU.mult)
            sop("dve", "tensor_reduce", sm[:, LIN, 0:8], sm[:, SEL, 0:64].rearrange("p (g j) -> p j g", g=8), AX.X, ALU.add)
            sop("dve", "tensor_reduce", sm[:, M1, 0:1], sm[:, LIN, 0:8], AX.X, ALU.max)
            sop("dve", "tensor_scalar", sm[:, OH1, 0:8], sm[:, LIN, 0:8], sm[:, M1, 0:1], None, ALU.is_equal)
            sop("dve", "scalar_tensor_tensor", sm[:, LIN2, 0:8], sm[:, OH1, 0:8], -1e30, sm[:, LIN, 0:8], ALU.mult, ALU.add)
            sop("dve", "tensor_reduce", sm[:, M2, 0:1], sm[:, LIN2, 0:8], AX.X, ALU.max)
            sop("dve", "tensor_scalar", sm[:, OH2, 0:8], sm[:, LIN2, 0:8], sm[:, M2, 0:1], None, ALU.is_equal)
            sop("dve", "tensor_tensor", sm[:, DM, 0:1], sm[:, M2, 0:1], sm[:, M1, 0:1], ALU.subtract)
            sop("act", "activation", sm[:, E2, 0:1], sm[:, DM, 0:1], AF.Exp)
            sop("dve", "tensor_scalar", sm[:, DEN, 0:1], sm[:, E2, 0:1], 1.0, None, ALU.add)
            sop("dve", "reciprocal", sm[:, RDEN, 0:1], sm[:, DEN, 0:1])
            sop("dve", "tensor_tensor", sm[:, W1G, 0:1], sm[:, RDEN, 0:1], sm[:, GPROB, 0:1], ALU.mult)
            sop("dve", "tensor_tensor", sm[:, W2G, 0:1], sm[:, W1G, 0:1], sm[:, E2, 0:1], ALU.mult)
            P.op("pe", L("matmul", ps_rk[:, 0:8], tris[:, :], sm[:, OHG, 0:8], start=True, stop=True), reads=[tris, sm], writes=[ps_rk])
            P.op("pe", L("matmul", ps_cs[:, 0:8], ones2[:, :], sm[:, OHG, 0:8], start=True, stop=True), reads=[ones2, sm], writes=[ps_cs])
            sop("dve", "tensor_tensor", sm[:, RK, 0:8], ps_rk[:, 0:8], cntB[:, :], ALU.add)
            P.op("dve", L("tensor_tensor", cntB[:, :], cntB[:, :], ps_cs[:, 0:8], ALU.add), reads=[cntB, ps_cs, sm], writes=[cntB])
            sop("dve", "tensor_tensor", sm[:, SEL, 0:8], sm[:, OHG, 0:8], sm[:, RK, 0:8], ALU.mult)
            sop("dve", "tensor_reduce", sm[:, DM, 0:1], sm[:, SEL, 0:8], AX.X, ALU.add)
            sop("dve", "tensor_tensor", sm[:, SEL, 0:8], sm[:, OHG, 0:8], gcapB[:, :], ALU.mult)
            sop("dve", "tensor_reduce", sm[:, DST, 0:1], sm[:, SEL, 0:8], AX.X, ALU.add)
            sop("dve", "tensor_scalar", sm[:, VAL, 0:1], sm[:, DM, 0:1], float(GCAP), None, ALU.is_lt)
            sop("dve", "tensor_tensor", sm[:, DST, 0:1], sm[:, DST, 0:1], sm[:, DM, 0:1], ALU.add)
            sop("dve", "tensor_scalar", sm[:, DST, 0:1], sm[:, DST, 0:1], -BIGI, None, ALU.add)
            sop("dve", "tensor_tensor", sm[:, DST, 0:1], sm[:, DST, 0:1], sm[:, VAL, 0:1], ALU.mult)
            sop("dve", "tensor_scalar", sm[:, DST, 0:1], sm[:, DST, 0:1], BIGI, None, ALU.add)
            P.op("dve", L("tensor_copy", destAll[:, tt:tt + 1], sm[:, DST, 0:1]), reads=[sm], writes=[destAll])
            sop("dve", "tensor_scalar", sm[:, GW, 0:8], sm[:, OH1, 0:8], sm[:, W1G, 0:1], None, ALU.mult)
            sop("dve", "scalar_tensor_tensor", sm[:, GW, 0:8], sm[:, OH2, 0:8], sm[:, W2G, 0:1], sm[:, GW, 0:8], ALU.mult, ALU.add)
            P.op("dve", L("tensor_scalar", FB[:, D:XW].bitcast(F32), sm[:, GW, 0:8], sm[:, VAL, 0:1], None, ALU.mult), reads=[sm], writes=[FB])
            P.dma("pool", None, None, reads=[FB, destAll], writes=[Xs],
                  fn=L("indirect_dma_start", out=Xs[:, :], out_offset=bass.IndirectOffsetOnAxis(ap=destAll[:, tt:tt + 1], axis=0),
                       in_=FB[:, :], in_offset=None, bounds_check=8 * GCAP - 1, oob_is_err=False))

        A.reset()
        W1 = [A.tile([128, 8, 512], BF16, f"W1_{i}") for i in range(4)]
        W3 = [A.tile([128, 8, 512], BF16, f"W3_{i}") for i in range(4)]
        W2 = [A.tile([128, 4, 1024], BF16, f"W2_{i}") for i in range(4)]
        xb = [A.tile([128, XW], BF16, f"xb{i}") for i in range(2)]
        xT = A.tile([128, 32, GCAP], BF16, "xT")
        gwt = A.tile([128, NGB, 8], F32, "gwt")
        s1 = A.tile([128, 512], F32, "s1")
        hb = A.tile([128, 512], BF16, "hb")
        hbT = A.tile([128, 4, 128], BF16, "hbT")
        Yacc = [A.tile([128, D], F32, f"Yacc{i}") for i in range(NGB)]
        pstb = [A.psum(i, 1, BF16, [128, 4, 128], f"pstb{i}") for i in range(2)]
        ps_h1 = A.psum(2, 1, F32, None, "ps_h1")
        ps_h3 = A.psum(3, 1, F32, None, "ps_h3")
        ps_y = [A.psum(4 + i, 1, F32, None, f"ps_y{i}") for i in range(3)]
        bi_ = 0
        for g_ in range(8):
            for b in range(NGB):
                XB = xb[bi_ % 2]; bi_ += 1
                r0 = g_ * GCAP + b * 128
                P.dma("sp", XB[:, :], Xs[r0:r0 + 128, :], reads=[Xs], writes=[XB])
                xTb = T(xT[:, :, b * 128:(b + 1) * 128], "xTb")
                xTb.w, xTb.r = None, {}
                for gq in range(8):
                    ps = pstb[gq % 2]
                    for j in range(4):
                        k = gq * 4 + j
                        P.op("pe", L("transpose", ps[:, j, :], XB[:, k * 128:(k + 1) * 128], identb[:, :]), reads=[XB, identb], writes=[ps])
                    if gq % 2 == 0:
                        P.op("dve", L("tensor_copy", xT[:, gq * 4:(gq + 1) * 4, b * 128:(b + 1) * 128], ps[:, :, :]), reads=[ps], writes=[xT])
                    else:
                        P.op("act", L("copy", xT[:, gq * 4:(gq + 1) * 4, b * 128:(b + 1) * 128], ps[:, :, :]), reads=[ps], writes=[xT])
                P.op("dve", L("tensor_copy", gwt[:, b, :], XB[:, D:XW].bitcast(F32)), reads=[XB], writes=[gwt])
            for j_ in range(8):
                e_ = g_ * 8 + j_
                for i in range(4):
                    P.dma("pool", W1[i][:, :, :], w1_d[e_, i * 1024:(i + 1) * 1024, :].rearrange("(k p) n -> p k n", p=128), reads=[w1_d], writes=[W1[i]])
                    P.dma("pool", W3[i][:, :, :], w3_d[e_, i * 1024:(i + 1) * 1024, :].rearrange("(k p) n -> p k n", p=128), reads=[w3_d], writes=[W3[i]])
                for i in range(4):
                    P.dma("pool", W2[i][:, :, :], w2_d[e_, :, i * 1024:(i + 1) * 1024].rearrange("(k p) n -> p k n", p=128), reads=[w2_d], writes=[W2[i]])
                for b in range(NGB):
                    for (psh, Wx) in ((ps_h1, W1), (ps_h3, W3)):
                        for k in range(32):
                            P.op("pe", L("matmul", psh[:, :], xT[:, k, b * 128:(b + 1) * 128], Wx[k // 8][:, k % 8, :], start=(k == 0), stop=(k == 31)),
                                 reads=[xT, Wx[k // 8]], writes=[psh])
                    P.op("act", L("activation", s1[:, :], ps_h1[:, :], AF.Silu), reads=[ps_h1], writes=[s1])
                    P.op("dve", L("tensor_tensor", hb[:, :], s1[:, :], ps_h3[:, :], ALU.mult), reads=[s1, ps_h3], writes=[hb])
                    for j in range(4):
                        P.op("pe", L("transpose", pstb[0][:, j, :], hb[:, j * 128:(j + 1) * 128], identb[:, :]), reads=[hb, identb], writes=[pstb[0]])
                    P.op("act", L("copy", hbT[:, :, :], pstb[0][:, :, :]), reads=[pstb[0]], writes=[hbT])
                    YA = Yacc[b]
                    for nb in range(8):
                        py = ps_y[nb % 3]
                        for j in range(4):
                            P.op("pe", L("matmul", py[:, :], hbT[:, j, :], W2[nb // 2][:, j, (nb % 2) * 512:(nb % 2) * 512 + 512], start=(j == 0), stop=(j == 3)),
                                 reads=[hbT, W2[nb // 2]], writes=[py])
                        ysl = YA[:, nb * 512:(nb + 1) * 512]
                        if j_ == 0:
                            P.op("dve", L("tensor_scalar", ysl, py[:, :], gwt[:, b, 0:1], None, ALU.mult), reads=[py, gwt], writes=[YA])
                        else:
                            P.op("dve", L("scalar_tensor_tensor", ysl, py[:, :], gwt[:, b, j_:j_ + 1], ysl, ALU.mult, ALU.add), reads=[py, gwt, YA], writes=[YA])
            for b in range(NGB):
                r0 = g_ * GCAP + b * 128
                P.dma("act", Yd[r0:r0 + 128, :], Yacc[b][:, :], reads=[Yacc[b]], writes=[Yd])

        A.reset()
        nfinB = A.tile([128, D], F32, "nfinB")
        P.dma("sp", nfinB[:, :], nfin_d[0:1, :].partition_broadcast(128), writes=[nfinB])
        hx = [A.tile([128, D], F32, f"hc{i}") for i in range(2)]
        yg = [A.tile([128, D], F32, f"yg{i}") for i in range(2)]
        ot = [A.tile([128, D], F32, f"ot{i}") for i in range(2)]
        junk6 = A.tile([128, D], BF16, "junk6c")
        ss6 = A.tile([128, 8], F32, "ss6c")
        for tt in range(16):
            X = hx[tt % 2]; Yg = yg[tt % 2]; O = ot[tt % 2]
            P.dma("sp", X[:, :], hmid[tt * 128:(tt + 1) * 128, :], reads=[hmid], writes=[X])
            P.op("pool", L("memset", Yg[:, :], 0.0), writes=[Yg])
            P.dma("pool", None, None, reads=[Yd, destAll], writes=[Yg],
                  fn=L("indirect_dma_start", out=Yg[:, :], out_offset=None, in_=Yd[:, :],
                       in_offset=bass.IndirectOffsetOnAxis(ap=destAll[:, tt:tt + 1], axis=0), bounds_check=8 * GCAP - 1, oob_is_err=False))
            P.op("dve", L("tensor_tensor", X[:, :], X[:, :], Yg[:, :], ALU.add), reads=[Yg, X], writes=[X])
            rms6(X, nfinB, O, junk6, ss6)
            P.dma("act", out[tt * 128:(tt + 1) * 128, :], O[:, :], reads=[O], writes=[out])

    P.finish()
    P.emit()
    return nc


def _prep_s5(inp, dd):
    f = np.float32
    are, aim = inp["s5_a_re"][0][dd], inp["s5_a_im"][0][dd]
    ldt = inp["s5_log_dt"][0][dd]
    bre, bim = inp["s5_b_re"][0][dd], inp["s5_b_im"][0][dd]
    cre, cim = inp["s5_c_re"][0][dd], inp["s5_c_im"][0][dd]
    q = np.arange(128); hf = q // 64; j = (q % 64) // 16; h = q % 16
    blk = np.arange(32); c = blk // 2; pr = blk % 2
    col = np.arange(128); two = col // 64; p = col % 64
    gA = 8 * c[None, :, None] + 4 * hf[:, None, None] + 2 * pr[None, :, None] + two[None, None, :]
    pA = np.broadcast_to(p[None, None, :], gA.shape)
    gB = np.broadcast_to(8 * c[None, :, None] + 4 * hf[:, None, None] + j[:, None, None], gA.shape)
    hB = np.broadcast_to(h[:, None, None], gA.shape)
    valid = (j[:, None, None] // 2 == pr[None, :, None]) & (j[:, None, None] % 2 == two[None, None, :])
    r = np.arange(128); two_r = r // 64; p_r = r % 64
    un = np.arange(64); c_u = un // 4; hf_u = (un % 4) // 2; pr_u = un % 2
    gS = 8 * c_u[None, :] + 4 * hf_u[None, :] + 2 * pr_u[None, :] + two_r[:, None]
    pS = np.broadcast_to(p_r[:, None], gS.shape)
    oc = np.arange(64); prp = oc // 32; twop = (oc % 32) // 16; hh = oc % 16
    validC = (prp[None, None, :] == pr_u[None, :, None]) & (twop[None, None, :] == two_r[:, None, None])
    gC = np.broadcast_to(gS[:, :, None], validC.shape)
    pC = np.broadcast_to(p_r[:, None, None], validC.shape)
    hC = np.broadcast_to(hh[None, None, :], validC.shape)
    s5A = np.zeros((2, 3, 128, 32, 128), f); s5B = np.zeros((2, 2, 128, 32, 128), f)
    s5S = np.zeros((2, 3, 128, 64), f); s5C = np.zeros((2, 2, 128, 64, 64), f)
    for d in range(2):
        s5A[d, 0] = are[d][gA, pA]; s5A[d, 1] = aim[d][gA, pA]; s5A[d, 2] = ldt[d][gA]
        s5B[d, 0] = np.where(valid, bre[d][gB, pA, hB], 0); s5B[d, 1] = np.where(valid, bim[d][gB, pA, hB], 0)
        s5S[d, 0] = are[d][gS, pS]; s5S[d, 1] = aim[d][gS, pS]; s5S[d, 2] = ldt[d][gS]
        s5C[d, 0] = np.where(validC, cre[d][gC, hC, pC], 0); s5C[d, 1] = np.where(validC, cim[d][gC, hC, pC], 0)
    s5d = np.ascontiguousarray(inp["s5_d"][0].reshape(16, 128).T)
    return {"s5A": s5A, "s5B": s5B, "s5S": s5S, "s5C": s5C, "s5d": s5d}


def make_in_maps(inp, cores=range(NCORES)):
    f = np.float32
    consts = {"ident": np.eye(128, dtype=f),
              "iota": np.ascontiguousarray(np.broadcast_to(np.arange(1024, dtype=f)[None, :], (128, 1024)))}
    r_ = np.arange(128)
    same = (r_[:, None] // 64) == (r_[None, :] // 64)
    s_, t_ = r_[:, None], r_[None, :]
    consts["tri"] = np.stack([same & (s_ <= t_), same & (s_ > t_), same & (s_ >= t_), same & (s_ < t_)]).astype(f)
    r6 = np.arange(64)
    consts["mask"] = np.stack([r6[:, None] <= r6[None, :], r6[:, None] >= r6[None, :]]).astype(f)
    consts["hg_norm_w"] = inp["hg_norm_w"]
    consts["s5_glu_w"] = inp["s5_glu_w"][0]
    consts["glub"] = np.ascontiguousarray(inp["s5_glu_b"][0].reshape(16, 128).T)
    consts["w_branch_a"] = inp["w_branch_a"][0]
    consts["w_branch_b"] = inp["w_branch_b"][0]
    consts["w_out"] = inp["w_out"][0]
    consts["norm_ffn_w"] = inp["norm_ffn_w"]
    consts["rw"] = np.concatenate([inp["router_group_w"][0], inp["router_expert_w"][0]], axis=1)
    consts["rb"] = np.concatenate([inp["router_group_b"][0], inp["router_expert_b"][0]])[None, :]
    consts["gcap"] = (np.arange(8) * 384).astype(f)[None, :]
    consts["tris"] = (r_[:, None] < r_[None, :]).astype(f)
    consts["expert_w1"] = inp["expert_w1"][0]
    consts["expert_w3"] = inp["expert_w3"][0]
    consts["expert_w2"] = inp["expert_w2"][0]
    consts["norm_final_w"] = inp["norm_final_w"][None, :]
    w_in_e = np.ascontiguousarray(inp["w_in"][0])
    w_in_o = np.concatenate([w_in_e[:, :6144], w_in_e[:, 8192:10240], w_in_e[:, 6144:8192], w_in_e[:, 10240:]], axis=1)
    par = []
    for odd in (0, 1):
        dd = [1, 0] if odd else [0, 1]
        m = dict(consts)
        m["w_in"] = w_in_o if odd else w_in_e
        m["norm_mix_w"] = inp["norm_mix_w"]
        m["lbg"] = np.ascontiguousarray(inp["hg_lb_gamma"][dd])
        m.update(_prep_s5(inp, dd))
        par.append(m)
    maps = []
    meta = inp["meta_tokens"]
    z16 = np.zeros_like(meta)
    for cidx in cores:
        b, odd = cidx // 2, cidx % 2
        x = inp["x"][b]
        m = dict(par[odd])
        if not odd:
            m["x_own"] = np.ascontiguousarray(x[:TOWN]); m["x_for"] = np.ascontiguousarray(x[TOWN:])
            m["x_pre"] = np.concatenate([meta, z16], 0)
        else:
            m["x_own"] = np.ascontiguousarray(x[TOWN:][::-1]); m["x_for"] = np.ascontiguousarray(x[:TOWN][::-1])
            m["x_pre"] = np.concatenate([z16, np.ascontiguousarray(meta[::-1])], 0)
        maps.append(m)
    return maps


def kernel(**inputs):
    inp = {k: np.asarray(v) for k, v in inputs.items()}
    nc = build()
    maps = make_in_maps(inp)
    res = run_bass_kernel_spmd(nc, maps, core_ids=list(range(NCORES)))
    outp = np.zeros((4, 4096, D), np.float32)
    for cidx in range(NCORES):
        b, odd = cidx // 2, cidx % 2
        o = res.results[cidx]["out"]
        if not odd:
            outp[b, :TOWN] = o
        else:
            outp[b, TOWN:] = o[::-1]
    return outp
```

```python
import numpy as np
import concourse.bass as bass
import concourse.mybir as mybir
from concourse.bass_utils import run_bass_kernel_spmd

F32 = mybir.dt.float32
BF16 = mybir.dt.bfloat16
I32 = mybir.dt.int32
ALU = mybir.AluOpType
AF = mybir.ActivationFunctionType
AX = mybir.AxisListType

D = 4096
DH = 2048
NCORES = 8
TOWN = 2048
NPRE = 16


class T:
    def __init__(self, t, name):
        self.t = t
        self.name = name
        self.w = None
        self.r = {}

    def __getitem__(self, k):
        return self.t[k]


class Prog:
    ENGS = ["pe", "dve", "act", "pool", "sp"]

    def __init__(self, nc):
        self.nc = nc
        self.items = {e: [] for e in self.ENGS}
        self.seq = {e: 0 for e in self.ENGS}
        self.sem = {e: nc.alloc_semaphore("s_" + e) for e in self.ENGS}
        self.seen = {e: {} for e in self.ENGS}
        self.lanes = {}
        self.lane_rr = {}
        for q, n in (("sp", 8), ("act", 4), ("pool", 8)):
            self.lanes[q] = [[nc.alloc_semaphore(f"l_{q}{i}"), 0, (q, i)] for i in range(n)]
            self.lane_rr[q] = 0
        self.n = 0

    def sb(self, name, shape, dt):
        return T(self.nc.alloc_sbuf_tensor(name, list(shape), dt), name)

    def ps(self, name, shape, dt=F32):
        return T(self.nc.alloc_psum_tensor(name, list(shape), dt), name)

    def dram(self, name, shape, dt, kind="Internal"):
        return T(self.nc.dram_tensor(name, list(shape), dt, kind=kind).ap(), name)

    def _wait(self, eng, dep):
        kind, key, val = dep
        if kind == "eng":
            if key == eng and eng == "pe":
                return
            k = ("e", key)
            if self.seen[eng].get(k, 0) >= val:
                return
            self.seen[eng][k] = val
            self.items[eng].append(("wait", self.sem[key], val))
        else:
            q, i = key
            k = ("l", key)
            if self.seen[eng].get(k, 0) >= val:
                return
            self.seen[eng][k] = val
            self.items[eng].append(("wait", self.lanes[q][i][0], 16 * val))

    def _deps(self, eng, reads, writes):
        for r in reads:
            if r.w is not None:
                self._wait(eng, r.w)
        for w in writes:
            if w.w is not None:
                self._wait(eng, w.w)
            for k, v in w.r.items():
                self._wait(eng, (k[0], k[1], v))

    def _mark(self, tag, reads, writes):
        for r in reads:
            r.r[(tag[0], tag[1])] = tag[2]
        for w in writes:
            w.w = tag
            w.r = {}

    def op(self, eng, fn, reads=(), writes=()):
        self._deps(eng, reads, writes)
        self.seq[eng] += 1
        self.items[eng].append(("ins", fn, self.sem[eng], 1))
        self._mark(("eng", eng, self.seq[eng]), reads, writes)
        self.n += 1

    def dma(self, q, out_ap, in_ap, reads=(), writes=(), fn=None):
        lanes = self.lanes[q]
        li = self.lane_rr[q]
        self.lane_rr[q] = (li + 1) % len(lanes)
        lane = lanes[li]
        if lane[1] > 0:
            self._wait(q, ("lane", lane[2], lane[1]))
        self._deps(q, reads, writes)
        lane[1] += 1
        if fn is None:
            fn = lambda e, o=out_ap, i=in_ap: e.dma_start(out=o, in_=i)
        self.items[q].append(("ins", fn, lane[0], 16))
        self._mark(("lane", lane[2], lane[1]), reads, writes)
        self.n += 1

    def finish(self):
        for q in ("sp", "act", "pool"):
            for lane in self.lanes[q]:
                if lane[1] > 0:
                    self._wait("sp", ("lane", lane[2], lane[1]))
        for e in self.ENGS:
            if e != "sp" and self.seq[e] > 0:
                self._wait("sp", ("eng", e, self.seq[e]))

    def emit(self):
        nc = self.nc
        items = self.items

        def run(e, lst):
            for it in lst:
                if it[0] == "wait":
                    e.wait_ge(it[1], it[2])
                else:
                    it[1](e).then_inc(it[2], it[3])

        with nc.Block() as block:
            @block.tensor
            def _(e):
                run(e, items["pe"])

            @block.vector
            def _(e):
                run(e, items["dve"])

            @block.scalar
            def _(e):
                run(e, items["act"])

            @block.gpsimd
            def _(e):
                run(e, items["pool"])

            @block.sync
            def _(e):
                run(e, items["sp"])


def prod(xs):
    r = 1
    for v in xs:
        r *= v
    return r


class Arena:
    def __init__(self, P, nfloat):
        self.P = P
        self.t = P.nc.alloc_sbuf_tensor("arena", [128, nfloat], F32)
        self.n = nfloat
        self.off = 0
        self.k = 0
        self.pp = P.nc.alloc_psum_tensor("psum_all", [128, 8, 512], F32)

    def reset(self):
        self.P.barrier()
        self.off = 0

    def tile(self, shape, dt, name=None):
        free = prod(shape[1:])
        nfl = free if dt in (F32, I32) else (free + 1) // 2
        nfl = (nfl + 7) // 8 * 8
        assert self.off + nfl <= self.n, f"arena overflow {name} {self.off + nfl} > {self.n}"
        ap = self.t[:, self.off:self.off + nfl]
        self.off += nfl
        if dt != F32:
            ap = ap.bitcast(dt)
        ap = ap[:, 0:free]
        if len(shape) == 3:
            ap = ap.rearrange("p (a b) -> p a b", a=shape[1])
        elif len(shape) == 4:
            ap = ap.rearrange("p (a b c) -> p a b c", a=shape[1], b=shape[2])
        self.k += 1
        return T(ap, name or f"t{self.k}")

    def psum(self, bank, nbanks=1, dt=F32, shape=None, name=None):
        ap = self.pp[:, bank:bank + nbanks, :].rearrange("p a b -> p (a b)")
        if dt != F32:
            ap = ap.bitcast(dt)
        if shape is not None:
            free = prod(shape[1:])
            ap = ap[:, 0:free]
            if len(shape) == 3:
                ap = ap.rearrange("p (a b) -> p a b", a=shape[1])
        self.k += 1
        return T(ap, name or f"ps{self.k}")


def _barrier(self):
    for e in self.ENGS:
        for o in self.ENGS:
            if o != e and self.seq[o] > 0:
                self._wait(e, ("eng", o, self.seq[o]))
        for q in ("sp", "act", "pool"):
            for lane in self.lanes[q]:
                if lane[1] > 0:
                    self._wait(e, ("lane", lane[2], lane[1]))


Prog.barrier = _barrier


def L(name, *args, **kw):
    return lambda e: getattr(e, name)(*args, **kw)


NTOK = 2 * TOWN + 2 * NPRE
CAP = 256
NEXP = 64

FAMS = [("u", 0, 2048, "fm", "all"), ("q", 2048, 2048, "tm", "own"), ("v", 4096, 2048, "tm", "all"),
        ("f1", 6144, 2048, "tm", "ownpre"), ("f2", 8192, 2048, "tm", "all"), ("go", 10240, 2048, "tm", "own"),
        ("ga", 12288, 4096, "fm", "own"), ("gb", 16384, 4096, "fm", "own")]


def build(debug=False, stages=99):
    nc = bass.Bass("TRN2", target_bir_lowering=False)
    P = Prog(nc)
    A = Arena(P, 50176)
    okind = "ExternalOutput" if debug else "Internal"

    def din(name, shape, dt=F32):
        return T(nc.dram_tensor(name, list(shape), dt, kind="ExternalInput").ap(), name)

    x_own = din("x_own", [TOWN, D])
    x_for = din("x_for", [TOWN, D])
    x_pre = din("x_pre", [2 * NPRE, D])
    nmw = din("norm_mix_w", [1, D])
    w_in = din("w_in", [D, 20480])
    ident_d = din("ident", [128, 128])
    lbg = din("lbg", [2, 2, DH])
    out = T(nc.dram_tensor("out", [TOWN, D], F32, kind="ExternalOutput").ap(), "out")
    iota_d = din("iota", [128, 1024])
    s5A_d = din("s5A", [2, 3, 128, 32, 128])
    s5B_d = din("s5B", [2, 2, 128, 32, 128])
    s5S_d = din("s5S", [2, 3, 128, 64])
    s5C_d = din("s5C", [2, 2, 128, 64, 64])
    s5d_d = din("s5d", [128, 16])
    tri_d = din("tri", [4, 128, 128])
    mask_d = din("mask", [2, 64, 64])
    hgnw_d = din("hg_norm_w", [1, DH])
    gluw_d = din("s5_glu_w", [DH, DH])
    glub_d = din("glub", [128, 16])
    wba_d = din("w_branch_a", [DH, D])
    wbb_d = din("w_branch_b", [DH, D])
    wout_d = din("w_out", [D, D])
    nfw_d = din("norm_ffn_w", [1, D])
    rw_d = din("rw", [D, 72])
    rb_d = din("rb", [1, 72])
    gcap_d = din("gcap", [1, 8])
    tris_d = din("tris", [128, 128])
    w1_d = din("expert_w1", [NEXP, D, 512])
    w3_d = din("expert_w3", [NEXP, D, 512])
    w2_d = din("expert_w2", [NEXP, 512, D])
    nfin_d = din("norm_final_w", [1, D])

    aT = P.dram("aT", [D, NTOK], BF16, okind)
    uT = P.dram("uT", [DH, NTOK], F32, okind)
    q_s = P.dram("q_s", [TOWN, DH], F32, okind)
    v_s = P.dram("v_s", [NTOK, DH], F32, okind)
    f1_s = P.dram("f1_s", [NTOK, DH], F32, okind)
    f2_s = P.dram("f2_s", [NTOK, DH], F32, okind)
    go_s = P.dram("go_s", [TOWN, DH], F32, okind)
    ga_s = P.dram("ga_s", [D, TOWN], F32, okind)
    gb_s = P.dram("gb_s", [D, TOWN], F32, okind)
    y1T = P.dram("y1T", [DH, TOWN], F32, okind)
    zT = P.dram("zT", [DH, TOWN], BF16, okind)
    o_s = P.dram("o_s", [TOWN, DH], F32, okind)
    hgT = P.dram("hgT", [DH, TOWN], BF16, okind)
    s5oT = P.dram("s5oT", [DH, TOWN], BF16, okind)
    mA_s = P.dram("mA_s", [D, TOWN], F32, okind)
    mT = P.dram("mT", [D, TOWN], BF16, okind)
    hmid = P.dram("hmid", [TOWN, D], F32, okind)
    Xs = P.dram("Xs", [8 * 384, D + 16], BF16)
    Yd = P.dram("Yd", [8 * 384, D], F32)
    fam_out = {"u": uT, "q": q_s, "v": v_s, "f1": f1_s, "f2": f2_s, "go": go_s, "ga": ga_s, "gb": gb_s}

    ident = T(nc.alloc_sbuf_tensor("ident_sb", [128, 128], F32), "ident")
    identb = T(nc.alloc_sbuf_tensor("identb", [128, 128], BF16), "identb")
    P.dma("sp", ident[:, :], ident_d[:, :], writes=[ident])
    P.op("dve", L("tensor_copy", identb[:, :], ident[:, :]), reads=[ident], writes=[identb])

    nmwB = A.tile([128, D], F32, "nmwB")
    P.dma("sp", nmwB[:, :], nmw[0:1, :].partition_broadcast(128), writes=[nmwB])
    xt = [A.tile([128, D], F32, f"xt{i}") for i in range(2)]
    ab = [A.tile([128, D], BF16, f"ab{i}") for i in range(2)]
    ss = [A.tile([128, 8], F32, f"ss{i}") for i in range(2)]
    junk = A.tile([128, D], BF16, "junk")
    aTt = [A.tile([128, 32, 128], BF16, f"aTt{i}") for i in range(2)]
    pst = [A.psum(i, 1, BF16, [128, 4, 128], f"pst{i}") for i in range(2)]

    def rms_tile(X, S, nrow, wB, Aout):
        P.op("act", L("activation", junk[:nrow, :], X[:nrow, :], AF.Square, accum_out=S[:nrow, 0:1]),
             reads=[X], writes=[junk, S])
        P.op("dve", L("tensor_scalar", S[:nrow, 1:2], S[:nrow, 0:1], 1.0 / D, 1e-6, ALU.mult, ALU.add),
             reads=[S], writes=[S])
        P.op("act", L("activation", S[:nrow, 2:3], S[:nrow, 1:2], AF.Sqrt), reads=[S], writes=[S])
        P.op("dve", L("reciprocal", S[:nrow, 3:4], S[:nrow, 2:3]), reads=[S], writes=[S])
        P.op("dve", L("scalar_tensor_tensor", Aout[:nrow, :], X[:nrow, :], S[:nrow, 3:4], wB[:nrow, :],
                                                     ALU.mult, ALU.mult), reads=[X, S, wB], writes=[Aout])

    def transpose_to(Asrc, nrow, AT, idt, pstiles):
        for g in range(8):
            ps = pstiles[g % 2]
            for j in range(4):
                k = g * 4 + j
                P.op("pe", L("transpose", ps[:, j, :nrow], Asrc[:nrow, k * 128:(k + 1) * 128],
                                                                  idt[:nrow, :nrow]), reads=[Asrc, idt], writes=[ps])
            if g % 2 == 0:
                P.op("dve", L("tensor_copy", AT[:, g * 4:(g + 1) * 4, :nrow], ps[:, :, :nrow]),
                     reads=[ps], writes=[AT])
            else:
                P.op("act", L("copy", AT[:, g * 4:(g + 1) * 4, :nrow], ps[:, :, :nrow]),
                     reads=[ps], writes=[AT])

    def norm_tile(src, r0, nrow, col0, i):
        X, Ab, S, AT = xt[i % 2], ab[i % 2], ss[i % 2], aTt[i % 2]
        P.dma("sp", X[:nrow, :], src[r0:r0 + nrow, :], writes=[X])
        rms_tile(X, S, nrow, nmwB, Ab)
        transpose_to(Ab, nrow, AT, identb, pst)
        P.dma("act", aT[:, col0:col0 + nrow].rearrange("(k p) t -> p k t", p=128), AT[:, :, :nrow],
              reads=[AT], writes=[aT])

    import os as _os
    SKIP = _os.environ.get("SKIP123")
    i = 0
    for tt in range(0 if SKIP else TOWN // 128):
        norm_tile(x_own, tt * 128, 128, tt * 128, i); i += 1
    for tt in range(0 if SKIP else TOWN // 128):
        norm_tile(x_for, tt * 128, 128, TOWN + tt * 128, i); i += 1
    norm_tile(x_pre, 0, 2 * NPRE, 2 * TOWN, i); i += 1

    def gemm(XT, K, col0, ncol, W, blocks, wb, epi):
        KC = K // 128
        xg = A.tile([128, KC, ncol], BF16, "xg")
        for k4 in range(0, KC, 8):
            P.dma("sp", xg[:, k4:k4 + 8, :], XT[k4 * 128:(k4 + 8) * 128, col0:col0 + ncol].rearrange("(k p) t -> p k t", p=128),
                  reads=[XT], writes=[xg])
        wbuf = [A.tile([128, KC, wb], BF16, f"wbuf{i}") for i in range(2)]
        pss = [A.psum(b, 1, F32, None, f"gps{b}") for b in range(6)]
        pi = 0
        for bi, (wc0, mode, need) in enumerate(blocks):
            Wt = wbuf[bi % 2]
            P.dma("pool", Wt[:, :, :], W[:, wc0:wc0 + wb].rearrange("(k p) n -> p k n", p=128), reads=[W], writes=[Wt])
            if mode == "fm":
                for m in range(wb // 128):
                    for n0 in range(0, ncol, 512):
                        nn = min(512, ncol - n0)
                        if not need(n0):
                            continue
                        ps = pss[pi % 6]; pi += 1
                        for k in range(KC):
                            P.op("pe", L("matmul", ps[:, :nn], Wt[:, k, m * 128:(m + 1) * 128], xg[:, k, n0:n0 + nn],
                                start=(k == 0), stop=(k == KC - 1)), reads=[Wt, xg], writes=[ps])
                        epi(ps, "fm", wc0 + m * 128, 128, col0 + n0, nn, xg)
            else:
                for t0 in range(0, ncol, 128):
                    rows = min(128, ncol - t0)
                    if not need(t0):
                        continue
                    ps = pss[pi % 6]; pi += 1
                    for k in range(KC):
                        P.op("pe", L("matmul", ps[:rows, :wb], xg[:, k, t0:t0 + rows], Wt[:, k, :],
                            start=(k == 0), stop=(k == KC - 1)), reads=[Wt, xg], writes=[ps])
                    epi(ps, "tm", wc0, wb, col0 + t0, rows, xg)

    if stages >= 2 and not SKIP:
        A.reset()
        lbB = [A.tile([128, DH], F32, f"lbB{d}") for d in range(2)]
        mark0 = A.off
        tmpg = A.tile([128, DH], F32, "tmpg")
        for d in range(2):
            P.dma("sp", lbB[d][:, :], lbg[d, 0:1, :].partition_broadcast(128), writes=[lbB[d]])
            P.dma("sp", tmpg[:, :], lbg[d, 1:2, :].partition_broadcast(128), writes=[tmpg])
            P.op("dve", L("tensor_tensor", lbB[d][:, :], lbB[d][:, :], tmpg[:, :], ALU.subtract),
                 reads=[lbB[d], tmpg], writes=[lbB[d]])
            P.op("act", L("activation", lbB[d][:, :], lbB[d][:, :], AF.Sigmoid), reads=[lbB[d]], writes=[lbB[d]])
        P.barrier(); A.off = mark0
        stg = [A.tile([128, 512], F32, f"stg{i}") for i in range(4)]
        si = [0]

        def proj_epi(ps, mode, c0, nc_, t0, nt, xg):
            fam = [f for f in FAMS if f[1] <= c0 < f[1] + f[2]][0]
            name, fc0 = fam[0], fam[1]
            S = stg[si[0] % 4]; si[0] += 1
            dst = fam_out[name]
            if mode == "fm":
                r0 = c0 - fc0
                if name == "u":
                    P.op("dve", L("tensor_copy", S[:, :nt], ps[:, :nt]), reads=[ps], writes=[S])
                else:
                    P.op("act", L("activation", S[:, :nt], ps[:, :nt], AF.Sigmoid), reads=[ps], writes=[S])
                P.dma("act", dst[r0:r0 + 128, t0:t0 + nt], S[:, :nt], reads=[S], writes=[dst])
            else:
                cc = c0 - fc0
                if name in ("q", "go"):
                    P.op("act", L("activation", S[:nt, :nc_], ps[:nt, :nc_], AF.Silu), reads=[ps], writes=[S])
                elif name == "v":
                    P.op("dve", L("tensor_copy", S[:nt, :nc_], ps[:nt, :nc_]), reads=[ps], writes=[S])
                else:
                    d = 0 if name == "f1" else 1
                    P.op("act", L("activation", S[:nt, :nc_], ps[:nt, :nc_], AF.Sigmoid), reads=[ps], writes=[S])
                    S2 = stg[si[0] % 4]; si[0] += 1
                    P.op("dve", L("tensor_scalar", S2[:nt, :nc_], S[:nt, :nc_], -1.0, 1.0, ALU.mult, ALU.add),
                         reads=[S], writes=[S2])
                    P.op("dve", L("tensor_tensor", S2[:nt, :nc_], S2[:nt, :nc_], lbB[d][:nt, cc:cc + nc_], ALU.mult),
                         reads=[S2, lbB[d]], writes=[S2])
                    P.op("dve", L("tensor_tensor", S[:nt, :nc_], S[:nt, :nc_], S2[:nt, :nc_], ALU.add),
                         reads=[S, S2], writes=[S])
                P.dma("act", dst[t0:t0 + nt, cc:cc + nc_], S[:nt, :nc_], reads=[S], writes=[dst])

        WB = 256
        blocks_own, blocks_for = [], []
        for (name, fc0, fn, mode, scope) in FAMS:
            for b in range(fn // WB):
                blocks_own.append((fc0 + b * WB, mode, lambda t: True))
                if scope == "all":
                    blocks_for.append((fc0 + b * WB, mode, lambda t: True))
                elif scope == "ownpre":
                    blocks_for.append((fc0 + b * WB, mode, lambda t: t >= TOWN))
        mark = A.off
        gemm(aT, D, 0, TOWN, w_in, blocks_own, WB, proj_epi)
        P.barrier(); A.off = mark
        gemm(aT, D, TOWN, TOWN + 2 * NPRE, w_in, blocks_for, WB, proj_epi)


    if stages >= 3 and not SKIP:
        A.reset()
        TWO_PI = float(2 * np.pi)
        io = A.tile([128, 1024], F32, "io")
        P.dma("sp", io[:, :], iota_d[:, :], writes=[io])
        s5dt = A.tile([128, 16], F32, "s5dt")
        P.dma("sp", s5dt[:, :], s5d_d[:, :], writes=[s5dt])
        seg_defs = [
            [(4096, 16, 1.0, 1.0, -1), (0, 1024, 1.0, 17.0, 0), (1024, 1024, 1.0, 1041.0, 1)],
            [(4112, 16, -1.0, 16.0, -1), (3072, 1024, -1.0, 1040.0, -1), (2048, 1024, -1.0, 2064.0, -1),
             (1024, 1024, -1.0, 3088.0, 1), (0, 1024, -1.0, 4112.0, 0)],
        ]
        mark_d = A.off
        for d in range(2):
            P.barrier(); A.off = mark_d
            segs = seg_defs[d]
            WBr = A.tile([128, 32, 128], BF16, "WBr")
            WBi = A.tile([128, 32, 128], BF16, "WBi")
            Cwr = A.tile([128, 64, 64], BF16, "Cwr")
            Cwi = A.tile([128, 64, 64], BF16, "Cwi")
            rho_s = A.tile([128, 64], F32, "rho_s")
            kap_s = A.tile([128, 64], F32, "kap_s")
            skap = A.tile([128, len(segs), 64], F32, "skap")
            bkap = A.tile([128, len(segs), 64], F32, "bkap")
            bkap25 = A.tile([128, len(segs), 64], F32, "bkap25")
            mark_setup = A.off
            tl = {n: A.tile([128, 1024], F32, "su_" + n) for n in
                  ["ar", "ai", "ld", "dt", "rho", "kap", "kf", "sn", "cs", "t1", "t2", "zr", "zi", "br", "bi"]}
            ki = A.tile([128, 1024], I32, "su_ki")

            def ew(eng, fn, reads, writes):
                P.op(eng, fn, reads=[tl[r] if isinstance(r, str) else r for r in reads],
                     writes=[tl[w] if isinstance(w, str) else w for w in writes])

            def TT(o, a, b, op, eng="dve"):
                ew(eng, L("tensor_tensor", tl[o][:, :], tl[a][:, :], tl[b][:, :], op), [a, b], [o])

            def sincos(kapname):
                for outn, shift in (("sn", 0.0), ("cs", 0.25)):
                    ew("dve", L("tensor_scalar", tl["t1"][:, :], tl[kapname][:, :], 1.0, shift, ALU.mult, ALU.add),
                       [kapname], ["t1"])
                    ew("dve", L("tensor_copy", ki[:, :], tl["t1"][:, :]), ["t1"], [ki])
                    ew("dve", L("tensor_copy", tl["kf"][:, :], ki[:, :]), [ki], ["kf"])
                    TT("t1", "t1", "kf", ALU.subtract)
                    ew("act", L("activation", tl[outn][:, :], tl["t1"][:, :], AF.Sin, scale=TWO_PI), ["t1"], [outn])

            for qd in range(4):
                bsl = slice(8 * qd, 8 * qd + 8)
                for w_, nm in enumerate(["ar", "ai", "ld"]):
                    P.dma("sp", tl[nm][:, :].rearrange("p (a b) -> p a b", a=8), s5A_d[d, w_, :, bsl, :], writes=[tl[nm]])
                for w_, nm in enumerate(["br", "bi"]):
                    P.dma("sp", tl[nm][:, :].rearrange("p (a b) -> p a b", a=8), s5B_d[d, w_, :, bsl, :], writes=[tl[nm]])
                ew("act", L("activation", tl["dt"][:, :], tl["ld"][:, :], AF.Exp), ["ld"], ["dt"])
                TT("t1", "ar", "dt", ALU.mult)
                ew("act", L("activation", tl["rho"][:, :], tl["t1"][:, :], AF.Exp), ["t1"], ["rho"])
                TT("kap", "ai", "dt", ALU.mult)
                ew("dve", L("tensor_scalar", tl["kap"][:, :], tl["kap"][:, :], 1.0 / TWO_PI, None, ALU.mult), ["kap"], ["kap"])
                sincos("kap")
                TT("cs", "rho", "cs", ALU.mult)
                TT("sn", "rho", "sn", ALU.mult)
                TT("t1", "ar", "ar", ALU.mult)
                TT("t2", "ai", "ai", ALU.mult)
                TT("t1", "t1", "t2", ALU.add)
                ew("dve", L("reciprocal", tl["t1"][:, :], tl["t1"][:, :]), ["t1"], ["t1"])
                ew("dve", L("tensor_scalar", tl["cs"][:, :], tl["cs"][:, :], -1.0, None, ALU.add), ["cs"], ["cs"])
                TT("zr", "cs", "ar", ALU.mult)
                TT("t2", "sn", "ai", ALU.mult)
                TT("zr", "zr", "t2", ALU.add)
                TT("zr", "zr", "t1", ALU.mult)
                TT("zi", "sn", "ar", ALU.mult)
                TT("t2", "cs", "ai", ALU.mult)
                TT("zi", "zi", "t2", ALU.subtract)
                TT("zi", "zi", "t1", ALU.mult)
                TT("t1", "zr", "br", ALU.mult)
                TT("t2", "zi", "bi", ALU.mult)
                ew("dve", L("tensor_tensor", WBr[:, bsl, :], tl["t1"][:, :].rearrange("p (a b) -> p a b", a=8),
                                                            tl["t2"][:, :].rearrange("p (a b) -> p a b", a=8), ALU.subtract),
                   ["t1", "t2"], [WBr])
                TT("t1", "zr", "bi", ALU.mult)
                TT("t2", "zi", "br", ALU.mult)
                ew("dve", L("tensor_tensor", WBi[:, bsl, :], tl["t1"][:, :].rearrange("p (a b) -> p a b", a=8),
                                                            tl["t2"][:, :].rearrange("p (a b) -> p a b", a=8), ALU.add),
                   ["t1", "t2"], [WBi])
            sA = {n: A.tile([128, 64], F32, "ss_" + n) for n in ["ar", "ai", "ld", "t"]}
            for w_, nm in enumerate(["ar", "ai", "ld"]):
                P.dma("sp", sA[nm][:, :], s5S_d[d, w_, :, :], writes=[sA[nm]])
            P.op("act", L("activation", sA["ld"][:, :], sA["ld"][:, :], AF.Exp), reads=[sA["ld"]], writes=[sA["ld"]])
            P.op("dve", L("tensor_tensor", sA["t"][:, :], sA["ar"][:, :], sA["ld"][:, :], ALU.mult),
                 reads=[sA["ar"], sA["ld"]], writes=[sA["t"]])
            P.op("act", L("activation", rho_s[:, :], sA["t"][:, :], AF.Exp), reads=[sA["t"]], writes=[rho_s])
            P.op("dve", L("tensor_tensor", kap_s[:, :], sA["ai"][:, :], sA["ld"][:, :], ALU.mult),
                 reads=[sA["ai"], sA["ld"]], writes=[kap_s])
            P.op("dve", L("tensor_scalar", kap_s[:, :], kap_s[:, :], 1.0 / TWO_PI, None, ALU.mult),
                 reads=[kap_s], writes=[kap_s])
            for si_, sg in enumerate(segs):
                P.op("dve", L("tensor_scalar", skap[:, si_, :], kap_s[:, :], sg[2], None, ALU.mult),
                     reads=[kap_s], writes=[skap])
                P.op("dve", L("tensor_scalar", bkap[:, si_, :], kap_s[:, :], sg[3], None, ALU.mult),
                     reads=[kap_s], writes=[bkap])
                P.op("dve", L("tensor_scalar", bkap25[:, si_, :], kap_s[:, :], sg[3], 0.25, ALU.mult, ALU.add),
                     reads=[kap_s], writes=[bkap25])
            for ri, Cw in enumerate([Cwr, Cwi]):
                for hh in range(4):
                    ctmp = tl["ar"]
                    P.dma("sp", ctmp[:, :].rearrange("p (a b) -> p a b", a=16), s5C_d[d, ri, :, 16 * hh:16 * hh + 16, :], writes=[ctmp])
                    P.op("dve", L("tensor_scalar", Cw[:, 16 * hh:16 * hh + 16, :], ctmp[:, :].rearrange("p (a b) -> p a b", a=16),
                        (1.0 if ri == 0 else -1.0), None, ALU.mult), reads=[ctmp], writes=[Cw])
            P.barrier(); A.off = mark_setup
            uf = A.tile([128, NTOK], F32, "uf")
            ub = A.tile([128, NTOK], BF16, "ub")
            csn = [[A.tile([128, 1024], F32, f"cs{i}"), A.tile([128, 1024], F32, f"sn{i}")] for i in range(2)]
            rr = A.tile([128, 1024], F32, "rr")
            rq = A.tile([128, 1024], F32, "rq")
            rf = A.tile([128, 1024], F32, "rf")
            ri_t = A.tile([128, 1024], I32, "ri")
            ri_t2 = A.tile([128, 1024], I32, "ri2")
            rf2 = A.tile([128, 1024], F32, "rf2")
            tt_ = [A.tile([128, 1024], F32, f"mt{i}") for i in range(4)]
            mm = A.tile([128, 2, 1024], F32, "mm")
            ww = A.tile([128, 2, 1024], F32, "ww")
            xr = A.tile([128, 1024], BF16, "xr")
            xi = A.tile([128, 1024], BF16, "xi")
            cr = A.tile([128, 2], F32, "cr")
            ys = [A.tile([128, 1024], F32, f"ys{i}") for i in range(2)]
            y1l = A.tile([128, 1024], F32, "y1l")
            zt = A.tile([128, 1024], BF16, "zt")
            bu_re = A.psum(0, 2, F32, None, "bu_re")
            bu_im = A.psum(2, 2, F32, None, "bu_im")
            psY = [A.psum(4, 2, F32, None, "psY0"), A.psum(6, 2, F32, None, "psY1")]
            tb = 0
            for c in range(16):
                P.dma("sp", uf[:, :], uT[c * 128:(c + 1) * 128, :], reads=[uT], writes=[uf])
                P.op("act", L("copy", ub[:, :], uf[:, :]), reads=[uf], writes=[ub])
                for hf in range(2):
                    hs = slice(hf * 64, hf * 64 + 64)
                    for pr in range(2):
                        un = c * 4 + hf * 2 + pr
                        blk = c * 2 + pr
                        first = True
                        for si_, (col0, n, sign, base, oseg) in enumerate(segs):
                            CS, SN = csn[tb % 2]; tb += 1
                            for TAB, src, ri_x, rf_x, bk in ((SN, rr, ri_t, rf, bkap), (CS, rq, ri_t2, rf2, bkap25)):
                                P.op("act", L("activation", src[:, :n], io[:, :n], AF.Identity, bias=bk[:, si_, un:un + 1], scale=skap[:, si_, un:un + 1]),
                                     reads=[io, skap, bk], writes=[src])
                                P.op("dve", L("tensor_copy", ri_x[:, :n], src[:, :n]), reads=[src], writes=[ri_x])
                                P.op("act", L("copy", rf_x[:, :n], ri_x[:, :n]), reads=[ri_x], writes=[rf_x])
                                P.op("dve", L("tensor_tensor", rf_x[:, :n], src[:, :n], rf_x[:, :n], ALU.subtract),
                                     reads=[src, rf_x], writes=[rf_x])
                                P.op("act", L("activation", TAB[:, :n], rf_x[:, :n], AF.Sin, scale=TWO_PI),
                                     reads=[rf_x], writes=[TAB])
                            for n0 in range(0, n, 512):
                                nn = min(512, n - n0)
                                for PSB, WBx in ((bu_re, WBr), (bu_im, WBi)):
                                    P.op("pe", L("matmul", PSB[:, n0:n0 + nn], WBx[hs, blk, :], ub[hs, col0 + n0:col0 + n0 + nn], start=True, stop=True),
                                        reads=[WBx, ub], writes=[PSB])
                            t1, t2, t3, t4 = tt_
                            P.op("dve", L("tensor_tensor", t1[:, :n], bu_re[:, :n], CS[:, :n], ALU.mult), reads=[bu_re, CS], writes=[t1])
                            P.op("dve", L("tensor_tensor", t2[:, :n], bu_im[:, :n], SN[:, :n], ALU.mult), reads=[bu_im, SN], writes=[t2])
                            P.op("dve", L("tensor_tensor", mm[:, 0, :n], t1[:, :n], t2[:, :n], ALU.add), reads=[t1, t2], writes=[mm])
                            P.op("dve", L("tensor_tensor", t3[:, :n], bu_im[:, :n], CS[:, :n], ALU.mult), reads=[bu_im, CS], writes=[t3])
                            P.op("dve", L("tensor_tensor", t4[:, :n], bu_re[:, :n], SN[:, :n], ALU.mult), reads=[bu_re, SN], writes=[t4])
                            P.op("dve", L("tensor_tensor", mm[:, 1, :n], t3[:, :n], t4[:, :n], ALU.subtract), reads=[t3, t4], writes=[mm])
                            for j in range(2):
                                init = 0.0 if first else cr[:, j:j + 1]
                                rb = rho_s[:, un:un + 1].to_broadcast([128, n])
                                if d == 0:
                                    P.op("dve", L("tensor_tensor_scan", ww[:, j, :n], rb, mm[:, j, :n], init, ALU.mult, ALU.add), reads=[mm, rho_s, cr], writes=[ww])
                                else:
                                    P.op("dve", L("tensor_tensor_scan", ww[:, j, 0:n][:, ::-1], rb, mm[:, j, 0:n][:, ::-1], init, ALU.mult, ALU.add),
                                        reads=[mm, rho_s, cr], writes=[ww])
                            last = n - 1 if d == 0 else 0
                            P.op("dve", L("tensor_copy", cr[:, :], ww[:, :, last]), reads=[ww], writes=[cr])
                            first = False
                            if oseg >= 0:
                                P.op("dve", L("tensor_tensor", t1[:, :n], ww[:, 0, :n], CS[:, :n], ALU.mult), reads=[ww, CS], writes=[t1])
                                P.op("dve", L("tensor_tensor", t2[:, :n], ww[:, 1, :n], SN[:, :n], ALU.mult), reads=[ww, SN], writes=[t2])
                                P.op("dve", L("tensor_tensor", xr[:, :n], t1[:, :n], t2[:, :n], ALU.subtract), reads=[t1, t2], writes=[xr])
                                P.op("dve", L("tensor_tensor", t3[:, :n], ww[:, 0, :n], SN[:, :n], ALU.mult), reads=[ww, SN], writes=[t3])
                                P.op("dve", L("tensor_tensor", t4[:, :n], ww[:, 1, :n], CS[:, :n], ALU.mult), reads=[ww, CS], writes=[t4])
                                P.op("dve", L("tensor_tensor", xi[:, :n], t3[:, :n], t4[:, :n], ALU.add), reads=[t3, t4], writes=[xi])
                                PY = psY[oseg]
                                for n0 in range(0, n, 512):
                                    P.op("pe", L("matmul", PY[hs, n0:n0 + 512], Cwr[:, un, :], xr[:, n0:n0 + 512], start=(pr == 0), stop=False),
                                        reads=[Cwr, xr], writes=[PY])
                                    P.op("pe", L("matmul", PY[hs, n0:n0 + 512], Cwi[:, un, :], xi[:, n0:n0 + 512], start=False, stop=(pr == 1)),
                                        reads=[Cwi, xi], writes=[PY])
                for oseg in range(2):
                    cols = slice(oseg * 1024, oseg * 1024 + 1024)
                    YS = ys[oseg]
                    if d == 0:
                        P.op("dve", L("scalar_tensor_tensor", YS[:, :], uf[:, cols], s5dt[:, c:c + 1], psY[oseg][:, :], ALU.mult, ALU.add),
                            reads=[uf, s5dt, psY[oseg]], writes=[YS])
                        P.dma("act", y1T[c * 128:(c + 1) * 128, cols], YS[:, :], reads=[YS], writes=[y1T])
                    else:
                        P.dma("sp", y1l[:, :], y1T[c * 128:(c + 1) * 128, cols], reads=[y1T], writes=[y1l])
                        P.op("dve", L("tensor_tensor", YS[:, :], psY[oseg][:, :], y1l[:, :], ALU.add),
                             reads=[psY[oseg], y1l], writes=[YS])
                        P.op("act", L("activation", zt[:, :], YS[:, :], AF.Gelu), reads=[YS], writes=[zt])
                        P.dma("act", zT[c * 128:(c + 1) * 128, cols], zt[:, :], reads=[zt], writes=[zT])

    if stages >= 4:
        A.reset()
        tri = A.tile([128, 4, 128], F32, "tri")
        P.dma("sp", tri[:, :, :], tri_d[:, :, :].rearrange("a p t -> p a t"), writes=[tri])
        msk = A.tile([64, 2, 64], F32, "msk")
        P.dma("sp", msk[:64, :, :], mask_d[:, :, :].rearrange("a p t -> p a t"), writes=[msk])
        ones = A.tile([128, 1], F32, "ones")
        P.op("dve", L("memset", ones[:, :], 1.0), writes=[ones])
        nwB = A.tile([64, DH], F32, "nwB")
        P.dma("sp", nwB[:64, :], hgnw_d[0:1, :].partition_broadcast(64), writes=[nwB])
        S = A.tile([128, 16, 128], F32, "S")
        Sb = A.tile([128, 16, 128], BF16, "Sb")
        ft = A.tile([128, DH], F32, "ft"); vt = A.tile([128, DH], F32, "vt"); qt = A.tile([128, DH], F32, "qt")
        gt = A.tile([128, DH], F32, "gt"); kt = A.tile([128, DH], F32, "kt")
        vb = A.tile([128, DH], BF16, "vb")
        v2 = A.tile([64, 2, DH], F32, "v2"); vb2 = A.tile([64, 2, DH], BF16, "vb2")
        esuf = A.tile([128, 512], F32, "esuf"); eb = A.tile([128, 512], F32, "eb"); enb = A.tile([128, 512], F32, "enb")
        kh = A.tile([128, 512], BF16, "kh"); qtl = A.tile([128, 512], BF16, "qtl"); ktl = A.tile([128, 512], BF16, "ktl")
        qkT = A.tile([128, 16, 64], BF16, "qkT")
        qkT8 = T(qkT[:, :, :].rearrange("p (a b) c -> p a (b c)", b=2), "qkT8v")
        sT = A.tile([64, 8, 64], BF16, "sT")
        ebt = A.tile([128, 4], F32, "ebt")
        o_sb = A.tile([64, 2, DH], F32, "o_sb")
        o1l = A.tile([64, 2, DH], F32, "o1l")
        gol = A.tile([64, 2, DH], F32, "gol")
        hgb = A.tile([64, 2, DH], BF16, "hgb")
        hT = A.tile([128, 16, 128], BF16, "hT")
        rs = A.tile([64, 4, 32], F32, "rs")
        ps_suf = A.psum(0, 1, F32, None, "ps_suf")
        ps_b = A.psum(1, 1, F32, None, "ps_b")
        ps_T = A.psum(2, 1, BF16, [128, 16, 64], "ps_T")
        ps_T8 = T(ps_T[:, :, :].rearrange("p (a b) c -> p a (b c)", b=2), "ps_T8v")
        ps_sc = A.psum(3, 1, F32, [128, 8, 64], "ps_sc")
        ps_o = A.psum(4, 2, F32, [128, 2, 512], "ps_o")
        ps_dS = A.psum(6, 1, F32, [128, 4, 128], "ps_dS")
        ps_bt = A.psum(7, 1, F32, None, "ps_bt")
        for d in range(2):
            fsrc = f1_s if d == 0 else f2_s
            P.op("dve", L("memset", S[:, :, :], 0.0), writes=[S])
            P.op("dve", L("memset", Sb[:, :, :], 0.0), writes=[Sb])
            if d == 0:
                tiles = [(4096, 16, False)] + [(128 * t_, 128, True) for t_ in range(16)]
            else:
                tiles = [(4112, 16, False)] + [(128 * t_, 128, False) for t_ in range(31, 15, -1)] + \
                        [(128 * t_, 128, True) for t_ in range(15, -1, -1)]
            import os as _os
            if _os.environ.get("HG_LIMIT"):
                lim = _os.environ["HG_LIMIT"].split(",")
                if d > int(lim[0]):
                    continue
                tiles = tiles[int(lim[1]):int(lim[2])]
            for (row0, nrows, is_out) in tiles:
                nch = (nrows + 63) // 64
                P.dma("sp", ft[:nrows, :], fsrc[row0:row0 + nrows, :], reads=[fsrc], writes=[ft])
                P.dma("sp", vt[:nrows, :], v_s[row0:row0 + nrows, :], reads=[v_s], writes=[vt])
                P.op("act", L("activation", gt[:nrows, :], ft[:nrows, :], AF.Ln), reads=[ft], writes=[gt])
                P.op("dve", L("tensor_scalar", kt[:nrows, :], ft[:nrows, :], -1.0, 1.0, ALU.mult, ALU.add), reads=[ft], writes=[kt])
                P.op("act", L("copy", vb[:nrows, :], vt[:nrows, :]), reads=[vt], writes=[vb])
                if is_out:
                    P.dma("sp", qt[:, :], q_s[row0:row0 + 128, :], reads=[q_s], writes=[qt])
                    P.dma("sp", v2[:64, :, :], v_s[row0:row0 + 128, :].rearrange("(c p) n -> p c n", p=64), reads=[v_s], writes=[v2])
                    P.op("act", L("copy", vb2[:64, :, :], v2[:64, :, :]), reads=[v2], writes=[vb2])
                for hg in range(4):
                    cs_ = slice(hg * 512, hg * 512 + 512)
                    P.op("pe", L("matmul", ps_suf[:nrows, :], tri[:nrows, 2 * d + 1, :nrows], gt[:nrows, cs_], start=True, stop=True),
                         reads=[tri, gt], writes=[ps_suf])
                    P.op("act", L("activation", esuf[:nrows, :], ps_suf[:nrows, :], AF.Exp), reads=[ps_suf], writes=[esuf])
                    P.op("dve", L("tensor_tensor", kh[:nrows, :], kt[:nrows, cs_], esuf[:nrows, :], ALU.mult), reads=[kt, esuf], writes=[kh])
                    if is_out:
                        P.op("pe", L("matmul", ps_b[:, :], tri[:, 2 * d, :], gt[:, cs_], start=True, stop=True), reads=[tri, gt], writes=[ps_b])
                        P.op("act", L("activation", eb[:, :], ps_b[:, :], AF.Exp), reads=[ps_b], writes=[eb])
                        P.op("act", L("activation", enb[:, :], ps_b[:, :], AF.Exp, scale=-1.0), reads=[ps_b], writes=[enb])
                        P.op("dve", L("tensor_tensor", qtl[:, :], qt[:, cs_], eb[:, :], ALU.mult), reads=[qt, eb], writes=[qtl])
                        P.op("dve", L("tensor_tensor", ktl[:, :], kt[:, cs_], enb[:, :], ALU.mult), reads=[kt, enb], writes=[ktl])
                        for which, src in enumerate([qtl, ktl]):
                            for hh in range(4):
                                P.op("pe", L("transpose", ps_T8[:, which * 4 + hh, :], src[:, hh * 128:(hh + 1) * 128], identb[:, :]),
                                     reads=[src, identb], writes=[ps_T])
                        P.op("act", L("copy", qkT8[:, :, :], ps_T8[:, :, :]), reads=[ps_T], writes=[qkT])
                        for idx in range(8):
                            cc_, hh_ = idx // 4, idx % 4
                            P.op("pe", L("matmul", ps_sc[:64, idx, :], qkT8[:, 4 + hh_, cc_ * 64:cc_ * 64 + 64], qkT8[:, hh_, cc_ * 64:cc_ * 64 + 64],
                                         start=True, stop=True), reads=[qkT], writes=[ps_sc])
                        P.op("dve", L("tensor_tensor", sT[:64, :, :], ps_sc[:64, :, :],
                                      msk[:64, d:d + 1, :].to_broadcast([64, 8, 64]), ALU.mult), reads=[ps_sc, msk], writes=[sT])
                    order = list(range(nch)) if d == 0 else list(range(nch - 1, -1, -1))
                    for cc in order:
                        r0 = 64 * cc
                        nr = min(64, nrows - r0)
                        for hh in range(4):
                            head = hg * 4 + hh
                            idx = cc * 4 + hh
                            hc = slice(head * 128, head * 128 + 128)
                            if is_out:
                                P.op("pe", L("matmul", ps_o[:64, cc, hh * 128:(hh + 1) * 128], qkT8[:, hh, cc * 64:cc * 64 + 64], Sb[:, head, :], start=True, stop=False),
                                     reads=[qkT, Sb], writes=[ps_o])
                                P.op("pe", L("matmul", ps_o[:64, cc, hh * 128:(hh + 1) * 128], sT[:64, idx, :], vb2[:64, cc, hc], start=False, stop=True),
                                     reads=[sT, vb2], writes=[ps_o])
                            P.op("pe", L("matmul", ps_dS[:, hh, :], kh[r0:r0 + nr, hh * 128:(hh + 1) * 128], vb[r0:r0 + nr, hc], start=True, stop=True),
                                 reads=[kh, vb], writes=[ps_dS])
                            P.op("pe", L("matmul", ps_bt[:, hh:hh + 1], gt[r0:r0 + nr, hc], ones[r0:r0 + nr, 0:1], start=True, stop=True),
                                 reads=[gt, ones], writes=[ps_bt])
                        P.op("act", L("activation", ebt[:, :], ps_bt[:, 0:4], AF.Exp), reads=[ps_bt], writes=[ebt])
                        for hh in range(4):
                            head = hg * 4 + hh
                            P.op("dve", L("scalar_tensor_tensor", S[:, head, :], S[:, head, :], ebt[:, hh:hh + 1], ps_dS[:, hh, :], ALU.mult, ALU.add),
                                 reads=[S, ebt, ps_dS], writes=[S])
                        P.op("act", L("copy", Sb[:, hg * 4:hg * 4 + 4, :], S[:, hg * 4:hg * 4 + 4, :]), reads=[S], writes=[Sb])
                    if is_out:
                        P.op("act", L("copy", o_sb[:64, :, cs_], ps_o[:64, :, :]), reads=[ps_o], writes=[o_sb])
                if not is_out:
                    continue
                o_dst = o_s[row0:row0 + 128, :].rearrange("(c p) n -> p c n", p=64)
                if d == 0:
                    P.dma("act", o_dst, o_sb[:64, :, :], reads=[o_sb], writes=[o_s])
                    continue
                P.dma("sp", o1l[:64, :, :], o_dst, reads=[o_s], writes=[o1l])
                P.dma("sp", gol[:64, :, :], go_s[row0:row0 + 128, :].rearrange("(c p) n -> p c n", p=64), reads=[go_s], writes=[gol])
                P.op("dve", L("tensor_tensor", o_sb[:64, :, :], o_sb[:64, :, :], o1l[:64, :, :], ALU.add), reads=[o_sb, o1l], writes=[o_sb])
                P.op("pool", L("tensor_tensor", o1l[:64, :, :], o_sb[:64, :, :], o_sb[:64, :, :], ALU.mult), reads=[o_sb], writes=[o1l])
                P.op("dve", L("tensor_reduce", rs[:64, 0, :], o1l[:64, :, :].rearrange("p c (h v) -> p (c h) v", v=128), AX.X, ALU.add),
                     reads=[o1l], writes=[rs])
                P.op("dve", L("tensor_scalar", rs[:64, 1, :], rs[:64, 0, :], 1.0 / 128, 1e-6, ALU.mult, ALU.add), reads=[rs], writes=[rs])
                P.op("act", L("activation", rs[:64, 2, :], rs[:64, 1, :], AF.Sqrt), reads=[rs], writes=[rs])
                P.op("dve", L("reciprocal", rs[:64, 3, :], rs[:64, 2, :]), reads=[rs], writes=[rs])
                P.op("dve", L("tensor_tensor", o_sb[:64, :, :].rearrange("p c (h v) -> p (c h) v", v=128),
                              o_sb[:64, :, :].rearrange("p c (h v) -> p (c h) v", v=128),
                              rs[:64, 3, :].unsqueeze(2).to_broadcast([64, 32, 128]), ALU.mult), reads=[o_sb, rs], writes=[o_sb])
                P.op("pool", L("tensor_tensor", o_sb[:64, :, :], o_sb[:64, :, :], nwB[:64, :].unsqueeze(1).to_broadcast([64, 2, DH]), ALU.mult),
                     reads=[o_sb, nwB], writes=[o_sb])
                P.op("dve", L("tensor_tensor", hgb[:64, :, :], o_sb[:64, :, :], gol[:64, :, :], ALU.mult), reads=[o_sb, gol], writes=[hgb])
                for cc in range(2):
                    for c16 in range(16):
                        P.op("pe", L("transpose", ps_T[:, c16, :], hgb[:64, cc, c16 * 128:(c16 + 1) * 128], identb[:64, :64]),
                             reads=[hgb, identb], writes=[ps_T])
                    P.op("act", L("copy", hT[:, :, cc * 64:(cc + 1) * 64], ps_T[:, :, :]), reads=[ps_T], writes=[hT])
                P.dma("act", hgT[:, row0:row0 + 128].rearrange("(c p) t -> p c t", p=128), hT[:, :, :], reads=[hT], writes=[hgT])

    if stages >= 5:
        A.reset()
        glub = A.tile([128, 16], F32, "glub")
        P.dma("sp", glub[:, :], glub_d[:, :], writes=[glub])
        stg5 = [A.tile([128, 512], F32, f"s5g{i}") for i in range(4)]
        stb5 = [A.tile([128, 512], BF16, f"s5b{i}") for i in range(4)]
        lg5 = [A.tile([128, 512], F32, f"s5l{i}") for i in range(4)]
        k5 = [0]

        def glu_epi(ps, mode, c0, nc_, t0, nt, xg):
            i = k5[0] % 4; k5[0] += 1
            m = c0 // 128
            P.op("act", L("activation", stg5[i][:, :nt], ps[:, :nt], AF.Sigmoid, bias=glub[:, m:m + 1]), reads=[ps, glub], writes=[stg5[i]])
            P.op("dve", L("tensor_tensor", stb5[i][:, :nt], stg5[i][:, :nt], xg[:, m, t0:t0 + nt], ALU.mult), reads=[stg5[i], xg], writes=[stb5[i]])
            P.dma("act", s5oT[c0:c0 + 128, t0:t0 + nt], stb5[i][:, :nt], reads=[stb5[i]], writes=[s5oT])

        gemm(zT, DH, 0, TOWN, gluw_d, [(b * 512, "fm", lambda t: True) for b in range(4)], 512, glu_epi)

        A.reset()
        stg5 = [A.tile([128, 512], F32, f"s5g{i}") for i in range(4)]
        stb5 = [A.tile([128, 512], BF16, f"s5b{i}") for i in range(4)]
        lg5 = [A.tile([128, 512], F32, f"s5l{i}") for i in range(4)]
        lh5 = [A.tile([128, 512], F32, f"s5h{i}") for i in range(4)]
        stage5_mark = [A.off]

        def bra_epi(ps, mode, c0, nc_, t0, nt, xg):
            i = k5[0] % 4; k5[0] += 1
            P.dma("sp", lg5[i][:, :nt], ga_s[c0:c0 + 128, t0:t0 + nt], reads=[ga_s], writes=[lg5[i]])
            P.op("dve", L("tensor_tensor", stg5[i][:, :nt], ps[:, :nt], lg5[i][:, :nt], ALU.mult), reads=[ps, lg5[i]], writes=[stg5[i]])
            P.dma("act", mA_s[c0:c0 + 128, t0:t0 + nt], stg5[i][:, :nt], reads=[stg5[i]], writes=[mA_s])

        gemm(s5oT, DH, 0, TOWN, wba_d, [(b * 512, "fm", lambda t: True) for b in range(8)], 512, bra_epi)

        P.barrier()

        def brb_epi(ps, mode, c0, nc_, t0, nt, xg):
            i = k5[0] % 4; k5[0] += 1
            P.dma("sp", lg5[i][:, :nt], gb_s[c0:c0 + 128, t0:t0 + nt], reads=[gb_s], writes=[lg5[i]])
            P.dma("sp", lh5[i][:, :nt], mA_s[c0:c0 + 128, t0:t0 + nt], reads=[mA_s], writes=[lh5[i]])
            P.op("dve", L("tensor_tensor", stg5[i][:, :nt], ps[:, :nt], lg5[i][:, :nt], ALU.mult), reads=[ps, lg5[i]], writes=[stg5[i]])
            P.op("dve", L("tensor_tensor", stb5[i][:, :nt], stg5[i][:, :nt], lh5[i][:, :nt], ALU.add), reads=[stg5[i], lh5[i]], writes=[stb5[i]])
            P.dma("act", mT[c0:c0 + 128, t0:t0 + nt], stb5[i][:, :nt], reads=[stb5[i]], writes=[mT])

        A.off = stage5_mark[0]
        gemm(hgT, DH, 0, TOWN, wbb_d, [(b * 512, "fm", lambda t: True) for b in range(8)], 512, brb_epi)

        A.reset()
        stg5 = [A.tile([128, 512], F32, f"s5g{i}") for i in range(4)]
        lg5 = [A.tile([128, 512], F32, f"s5l{i}") for i in range(4)]

        def wout_epi(ps, mode, c0, nc_, t0, nt, xg):
            i = k5[0] % 4; k5[0] += 1
            P.dma("sp", lg5[i][:nt, :nc_], x_own[t0:t0 + nt, c0:c0 + nc_], reads=[x_own], writes=[lg5[i]])
            P.op("dve", L("tensor_tensor", stg5[i][:nt, :nc_], ps[:nt, :nc_], lg5[i][:nt, :nc_], ALU.add), reads=[ps, lg5[i]], writes=[stg5[i]])
            P.dma("act", hmid[t0:t0 + nt, c0:c0 + nc_], stg5[i][:nt, :nc_], reads=[stg5[i]], writes=[hmid])

        gemm(mT, D, 0, TOWN, wout_d, [(b * 256, "tm", lambda t: True) for b in range(16)], 256, wout_epi)

    if stages >= 6:
        GCAP = 384
        NGB = GCAP // 128
        XW = D + 16
        BIGI = float(8 * GCAP + 1000)
        destAll = T(nc.alloc_sbuf_tensor("destAll", [128, 16], I32), "destAll")
        A.reset()
        nfwB = A.tile([128, D], F32, "nfwB")
        P.dma("sp", nfwB[:, :], nfw_d[0:1, :].partition_broadcast(128), writes=[nfwB])
        RWt = A.tile([128, 32, 72], F32, "RWt")
        P.dma("sp", RWt[:, :, :], rw_d[:, :].rearrange("(k p) n -> p k n", p=128), writes=[RWt])
        rbB = A.tile([128, 72], F32, "rbB")
        P.dma("sp", rbB[:, :], rb_d[0:1, :].partition_broadcast(128), writes=[rbB])
        gcapB = A.tile([128, 8], F32, "gcapB")
        P.dma("sp", gcapB[:, :], gcap_d[0:1, :].partition_broadcast(128), writes=[gcapB])
        tris = A.tile([128, 128], F32, "tris")
        P.dma("sp", tris[:, :], tris_d[:, :], writes=[tris])
        ones2 = A.tile([128, 128], F32, "ones2")
        P.op("dve", L("memset", ones2[:, :], 1.0), writes=[ones2])
        cntB = A.tile([128, 8], F32, "cntB")
        P.op("dve", L("memset", cntB[:, :], 0.0), writes=[cntB])
        hx = [A.tile([128, D], F32, f"hx{i}") for i in range(2)]
        Ff = A.tile([128, D], F32, "Ff")
        Fb = [A.tile([128, XW], BF16, f"Fb{i}") for i in range(2)]
        FT = A.tile([128, 32, 128], F32, "FT")
        junk6 = A.tile([128, D], BF16, "junk6")
        ss6 = A.tile([128, 8], F32, "ss6")
        sm = A.tile([128, 24, 64], F32, "sm")
        pstf = [A.psum(i, 1, F32, [128, 4, 128], f"pstf{i}") for i in range(2)]
        ps_lg = A.psum(2, 1, F32, None, "ps_lg")
        ps_rk = A.psum(3, 1, F32, None, "ps_rk")
        ps_cs = A.psum(4, 1, F32, None, "ps_cs")

        def rms6(X, wB, Aout, junk_, S_):
            P.op("act", L("activation", junk_[:, :], X[:, :], AF.Square, accum_out=S_[:, 0:1]), reads=[X], writes=[junk_, S_])
            P.op("dve", L("tensor_scalar", S_[:, 1:2], S_[:, 0:1], 1.0 / D, 1e-6, ALU.mult, ALU.add), reads=[S_], writes=[S_])
            P.op("act", L("activation", S_[:, 2:3], S_[:, 1:2], AF.Sqrt), reads=[S_], writes=[S_])
            P.op("dve", L("reciprocal", S_[:, 3:4], S_[:, 2:3]), reads=[S_], writes=[S_])
            P.op("dve", L("scalar_tensor_tensor", Aout[:, :], X[:, :], S_[:, 3:4], wB[:, :], ALU.mult, ALU.mult), reads=[X, S_, wB], writes=[Aout])

        def sop(eng, name, *args, **kw):
            P.op(eng, L(name, *args, **kw), reads=[sm, ps_lg, ps_rk, ps_cs, rbB, gcapB, cntB], writes=[sm])

        LG, GMAX, NGMAX, OHG, EG, GSUM, GPROB, SEL, LIN, M1, OH1, LIN2, M2, OH2, DM, E2, DEN, RDEN, W1G, W2G, GW, RK, VAL, DST = range(24)
        for tt in range(16):
            X = hx[tt % 2]; FB = Fb[tt % 2]
            P.dma("sp", X[:, :], hmid[tt * 128:(tt + 1) * 128, :], reads=[hmid], writes=[X])
            rms6(X, nfwB, Ff, junk6, ss6)
            P.op("pool", L("tensor_copy", FB[:, 0:D], Ff[:, :]), reads=[Ff], writes=[FB])
            transpose_to(Ff, 128, FT, ident, pstf)
            for k in range(32):
                P.op("pe", L("matmul", ps_lg[:, 0:72], FT[:, k, :], RWt[:, k, :], start=(k == 0), stop=(k == 31)), reads=[FT, RWt], writes=[ps_lg])
            sop("dve", "tensor_tensor", sm[:, LG, 0:64], ps_lg[:, 8:72], rbB[:, 8:72], ALU.add)
            sop("dve", "tensor_tensor", sm[:, EG, 0:8], ps_lg[:, 0:8], rbB[:, 0:8], ALU.add)
            sop("dve", "tensor_reduce", sm[:, GMAX, 0:1], sm[:, EG, 0:8], AX.X, ALU.max)
            sop("dve", "tensor_scalar", sm[:, OHG, 0:8], sm[:, EG, 0:8], sm[:, GMAX, 0:1], None, ALU.is_equal)
            sop("dve", "tensor_scalar", sm[:, NGMAX, 0:1], sm[:, GMAX, 0:1], -1.0, None, ALU.mult)
            sop("act", "activation", sm[:, EG, 8:16], sm[:, EG, 0:8], AF.Exp, bias=sm[:, NGMAX, 0:1], accum_out=sm[:, GSUM, 0:1])
            sop("dve", "reciprocal", sm[:, GPROB, 0:1], sm[:, GSUM, 0:1])
            sop("dve", "tensor_tensor", sm[:, SEL, 0:64].rearrange("p (g j) -> p g j", g=8), sm[:, LG, 0:64].rearrange("p (g j) -> p g j", g=8),
                sm[:, OHG, 0:8].unsqueeze(2).to_broadcast([128, 8, 8]), ALU.mult)
            sop("dve", "tensor_reduce", sm[:, LIN, 0:8], sm[:, SEL, 0:64].rearrange("p (g j) -> p j g", g=8), AX.X, ALU.add)
            sop("dve", "tensor_reduce", sm[:, M1, 0:1], sm[:, LIN, 0:8], AX.X, ALU.max)
            sop("dve", "tensor_scalar", sm[:, OH1, 0:8], sm[:, LIN, 0:8], sm[:, M1, 0:1], None, ALU.is_equal)
            sop("dve", "scalar_tensor_tensor", sm[:, LIN2, 0:8], sm[:, OH1, 0:8], -1e30, sm[:, LIN, 0:8], ALU.mult, ALU.add)
            sop("dve", "tensor_reduce", sm[:, M2, 0:1], sm[:, LIN2, 0:8], AX.X, ALU.max)
            sop("dve", "tensor_scalar", sm[:, OH2, 0:8], sm[:, LIN2, 0:8], sm[:, M2, 0:1], None, ALU.is_equal)
            sop("dve", "tensor_tensor", sm[:, DM, 0:1], sm[:, M2, 0:1], sm[:, M1, 0:1], ALU.subtract)
            sop("act", "activation", sm[:, E2, 0:1], sm[:, DM, 0:1], AF.Exp)
            sop("dve", "tensor_scalar", sm[:, DEN, 0:1], sm[:, E2, 0:1], 1.0, None, ALU.add)
            sop("dve", "reciprocal", sm[:, RDEN, 0:1], sm[:, DEN, 0:1])
            sop("dve", "tensor_tensor", sm[:, W1G, 0:1], sm[:, RDEN, 0:1], sm[:, GPROB, 0:1], ALU.mult)
            sop("dve", "tensor_tensor", sm[:, W2G, 0:1], sm[:, W1G, 0:1], sm[:, E2, 0:1], ALU.mult)
            P.op("pe", L("matmul", ps_rk[:, 0:8], tris[:, :], sm[:, OHG, 0:8], start=True, stop=True), reads=[tris, sm], writes=[ps_rk])
            P.op("pe", L("matmul", ps_cs[:, 0:8], ones2[:, :], sm[:, OHG, 0:8], start=True, stop=True), reads=[ones2, sm], writes=[ps_cs])
            sop("dve", "tensor_tensor", sm[:, RK, 0:8], ps_rk[:, 0:8], cntB[:, :], ALU.add)
            P.op("dve", L("tensor_tensor", cntB[:, :], cntB[:, :], ps_cs[:, 0:8], ALU.add), reads=[cntB, ps_cs, sm], writes=[cntB])
            sop("dve", "tensor_tensor", sm[:, SEL, 0:8], sm[:, OHG, 0:8], sm[:, RK, 0:8], ALU.mult)
            sop("dve", "tensor_reduce", sm[:, DM, 0:1], sm[:, SEL, 0:8], AX.X, ALU.add)
            sop("dve", "tensor_tensor", sm[:, SEL, 0:8], sm[:, OHG, 0:8], gcapB[:, :], ALU.mult)
            sop("dve", "tensor_reduce", sm[:, DST, 0:1], sm[:, SEL, 0:8], AX.X, ALU.add)
            sop("dve", "tensor_scalar", sm[:, VAL, 0:1], sm[:, DM, 0:1], float(GCAP), None, ALU.is_lt)
            sop("dve", "tensor_tensor", sm[:, DST, 0:1], sm[:, DST, 0:1], sm[:, DM, 0:1], ALU.add)
            sop("dve", "tensor_scalar", sm[:, DST, 0:1], sm[:, DST, 0:1], -BIGI, None, ALU.add)
            sop("dve", "tensor_tensor", sm[:, DST, 0:1], sm[:, DST, 0:1], sm[:, VAL, 0:1], ALU.mult)
            sop("dve", "tensor_scalar", sm[:, DST, 0:1], sm[:, DST, 0:1], BIGI, None, ALU.add)
            P.op("dve", L("tensor_copy", destAll[:, tt:tt + 1], sm[:, DST, 0:1]), reads=[sm], writes=[destAll])
            sop("dve", "tensor_scalar", sm[:, GW, 0:8], sm[:, OH1, 0:8], sm[:, W1G, 0:1], None, ALU.mult)
            sop("dve", "scalar_tensor_tensor", sm[:, GW, 0:8], sm[:, OH2, 0:8], sm[:, W2G, 0:1], sm[:, GW, 0:8], ALU.mult, ALU.add)
            P.op("dve", L("tensor_scalar", FB[:, D:XW].bitcast(F32), sm[:, GW, 0:8], sm[:, VAL, 0:1], None, ALU.mult), reads=[sm], writes=[FB])
            P.dma("pool", None, None, reads=[FB, destAll], writes=[Xs],
                  fn=L("indirect_dma_start", out=Xs[:, :], out_offset=bass.IndirectOffsetOnAxis(ap=destAll[:, tt:tt + 1], axis=0),
                       in_=FB[:, :], in_offset=None, bounds_check=8 * GCAP - 1, oob_is_err=False))

        A.reset()
        W1 = [A.tile([128, 8, 512], BF16, f"W1_{i}") for i in range(4)]
        W3 = [A.tile([128, 8, 512], BF16, f"W3_{i}") for i in range(4)]
        W2 = [A.tile([128, 4, 1024], BF16, f"W2_{i}") for i in range(4)]
        xb = [A.tile([128, XW], BF16, f"xb{i}") for i in range(2)]
        xT = A.tile([128, 32, GCAP], BF16, "xT")
        gwt = A.tile([128, NGB, 8], F32, "gwt")
        s1 = A.tile([128, 512], F32, "s1")
        hb = A.tile([128, 512], BF16, "hb")
        hbT = A.tile([128, 4, 128], BF16, "hbT")
        Yacc = [A.tile([128, D], F32, f"Yacc{i}") for i in range(NGB)]
        pstb = [A.psum(i, 1, BF16, [128, 4, 128], f"pstb{i}") for i in range(2)]
        ps_h1 = A.psum(2, 1, F32, None, "ps_h1")
        ps_h3 = A.psum(3, 1, F32, None, "ps_h3")
        ps_y = [A.psum(4 + i, 1, F32, None, f"ps_y{i}") for i in range(3)]
        bi_ = 0
        for g_ in range(8):
            for b in range(NGB):
                XB = xb[bi_ % 2]; bi_ += 1
                r0 = g_ * GCAP + b * 128
                P.dma("sp", XB[:, :], Xs[r0:r0 + 128, :], reads=[Xs], writes=[XB])
                xTb = T(xT[:, :, b * 128:(b + 1) * 128], "xTb")
                xTb.w, xTb.r = None, {}
                for gq in range(8):
                    ps = pstb[gq % 2]
                    for j in range(4):
                        k = gq * 4 + j
                        P.op("pe", L("transpose", ps[:, j, :], XB[:, k * 128:(k + 1) * 128], identb[:, :]), reads=[XB, identb], writes=[ps])
                    if gq % 2 == 0:
                        P.op("dve", L("tensor_copy", xT[:, gq * 4:(gq + 1) * 4, b * 128:(b + 1) * 128], ps[:, :, :]), reads=[ps], writes=[xT])
                    else:
                        P.op("act", L("copy", xT[:, gq * 4:(gq + 1) * 4, b * 128:(b + 1) * 128], ps[:, :, :]), reads=[ps], writes=[xT])
                P.op("dve", L("tensor_copy", gwt[:, b, :], XB[:, D:XW].bitcast(F32)), reads=[XB], writes=[gwt])
            for j_ in range(8):
                e_ = g_ * 8 + j_
                for i in range(4):
                    P.dma("pool", W1[i][:, :, :], w1_d[e_, i * 1024:(i + 1) * 1024, :].rearrange("(k p) n -> p k n", p=128), reads=[w1_d], writes=[W1[i]])
                    P.dma("pool", W3[i][:, :, :], w3_d[e_, i * 1024:(i + 1) * 1024, :].rearrange("(k p) n -> p k n", p=128), reads=[w3_d], writes=[W3[i]])
                for i in range(4):
                    P.dma("pool", W2[i][:, :, :], w2_d[e_, :, i * 1024:(i + 1) * 1024].rearrange("(k p) n -> p k n", p=128), reads=[w2_d], writes=[W2[i]])
                for b in range(NGB):
                    for (psh, Wx) in ((ps_h1, W1), (ps_h3, W3)):
                        for k in range(32):
                            P.op("pe", L("matmul", psh[:, :], xT[:, k, b * 128:(b + 1) * 128], Wx[k // 8][:, k % 8, :], start=(k == 0), stop=(k == 31)),
                                 reads=[xT, Wx[k // 8]], writes=[psh])
                    P.op("act", L("activation", s1[:, :], ps_h1[:, :], AF.Silu), reads=[ps_h1], writes=[s1])
                    P.op("dve", L("tensor_tensor", hb[:, :], s1[:, :], ps_h3[:, :], ALU.mult), reads=[s1, ps_h3], writes=[hb])
                    for j in range(4):
                        P.op("pe", L("transpose", pstb[0][:, j, :], hb[:, j * 128:(j + 1) * 128], identb[:, :]), reads=[hb, identb], writes=[pstb[0]])
                    P.op("act", L("copy", hbT[:, :, :], pstb[0][:, :, :]), reads=[pstb[0]], writes=[hbT])
                    YA = Yacc[b]
                    for nb in range(8):
                        py = ps_y[nb % 3]
                        for j in range(4):
                            P.op("pe", L("matmul", py[:, :], hbT[:, j, :], W2[nb // 2][:, j, (nb % 2) * 512:(nb % 2) * 512 + 512], start=(j == 0), stop=(j == 3)),
                                 reads=[hbT, W2[nb // 2]], writes=[py])
                        ysl = YA[:, nb * 512:(nb + 1) * 512]
                        if j_ == 0:
                            P.op("dve", L("tensor_scalar", ysl, py[:, :], gwt[:, b, 0:1], None, ALU.mult), reads=[py, gwt], writes=[YA])
                        else:
                            P.op("dve", L("scalar_tensor_tensor", ysl, py[:, :], gwt[:, b, j_:j_ + 1], ysl, ALU.mult, ALU.add), reads=[py, gwt, YA], writes=[YA])
            for b in range(NGB):
                r0 = g_ * GCAP + b * 128
                P.dma("act", Yd[r0:r0 + 128, :], Yacc[b][:, :], reads=[Yacc[b]], writes=[Yd])

        A.reset()
        nfinB = A.tile([128, D], F32, "nfinB")
        P.dma("sp", nfinB[:, :], nfin_d[0:1, :].partition_broadcast(128), writes=[nfinB])
        hx = [A.tile([128, D], F32, f"hc{i}") for i in range(2)]
        yg = [A.tile([128, D], F32, f"yg{i}") for i in range(2)]
        ot = [A.tile([128, D], F32, f"ot{i}") for i in range(2)]
        junk6 = A.tile([128, D], BF16, "junk6c")
        ss6 = A.tile([128, 8], F32, "ss6c")
        for tt in range(16):
            X = hx[tt % 2]; Yg = yg[tt % 2]; O = ot[tt % 2]
            P.dma("sp", X[:, :], hmid[tt * 128:(tt + 1) * 128, :], reads=[hmid], writes=[X])
            P.op("pool", L("memset", Yg[:, :], 0.0), writes=[Yg])
            P.dma("pool", None, None, reads=[Yd, destAll], writes=[Yg],
                  fn=L("indirect_dma_start", out=Yg[:, :], out_offset=None, in_=Yd[:, :],
                       in_offset=bass.IndirectOffsetOnAxis(ap=destAll[:, tt:tt + 1], axis=0), bounds_check=8 * GCAP - 1, oob_is_err=False))
            P.op("dve", L("tensor_tensor", X[:, :], X[:, :], Yg[:, :], ALU.add), reads=[Yg, X], writes=[X])
            rms6(X, nfinB, O, junk6, ss6)
            P.dma("act", out[tt * 128:(tt + 1) * 128, :], O[:, :], reads=[O], writes=[out])

    P.finish()
    P.emit()
    return nc


def _prep_s5(inp, dd):
    f = np.float32
    are, aim = inp["s5_a_re"][0][dd], inp["s5_a_im"][0][dd]
    ldt = inp["s5_log_dt"][0][dd]
    bre, bim = inp["s5_b_re"][0][dd], inp["s5_b_im"][0][dd]
    cre, cim = inp["s5_c_re"][0][dd], inp["s5_c_im"][0][dd]
    q = np.arange(128); hf = q // 64; j = (q % 64) // 16; h = q % 16
    blk = np.arange(32); c = blk // 2; pr = blk % 2
    col = np.arange(128); two = col // 64; p = col % 64
    gA = 8 * c[None, :, None] + 4 * hf[:, None, None] + 2 * pr[None, :, None] + two[None, None, :]
    pA = np.broadcast_to(p[None, None, :], gA.shape)
    gB = np.broadcast_to(8 * c[None, :, None] + 4 * hf[:, None, None] + j[:, None, None], gA.shape)
    hB = np.broadcast_to(h[:, None, None], gA.shape)
    valid = (j[:, None, None] // 2 == pr[None, :, None]) & (j[:, None, None] % 2 == two[None, None, :])
    r = np.arange(128); two_r = r // 64; p_r = r % 64
    un = np.arange(64); c_u = un // 4; hf_u = (un % 4) // 2; pr_u = un % 2
    gS = 8 * c_u[None, :] + 4 * hf_u[None, :] + 2 * pr_u[None, :] + two_r[:, None]
    pS = np.broadcast_to(p_r[:, None], gS.shape)
    oc = np.arange(64); prp = oc // 32; twop = (oc % 32) // 16; hh = oc % 16
    validC = (prp[None, None, :] == pr_u[None, :, None]) & (twop[None, None, :] == two_r[:, None, None])
    gC = np.broadcast_to(gS[:, :, None], validC.shape)
    pC = np.broadcast_to(p_r[:, None, None], validC.shape)
    hC = np.broadcast_to(hh[None, None, :], validC.shape)
    s5A = np.zeros((2, 3, 128, 32, 128), f); s5B = np.zeros((2, 2, 128, 32, 128), f)
    s5S = np.zeros((2, 3, 128, 64), f); s5C = np.zeros((2, 2, 128, 64, 64), f)
    for d in range(2):
        s5A[d, 0] = are[d][gA, pA]; s5A[d, 1] = aim[d][gA, pA]; s5A[d, 2] = ldt[d][gA]
        s5B[d, 0] = np.where(valid, bre[d][gB, pA, hB], 0); s5B[d, 1] = np.where(valid, bim[d][gB, pA, hB], 0)
        s5S[d, 0] = are[d][gS, pS]; s5S[d, 1] = aim[d][gS, pS]; s5S[d, 2] = ldt[d][gS]
        s5C[d, 0] = np.where(validC, cre[d][gC, hC, pC], 0); s5C[d, 1] = np.where(validC, cim[d][gC, hC, pC], 0)
    s5d = np.ascontiguousarray(inp["s5_d"][0].reshape(16, 128).T)
    return {"s5A": s5A, "s5B": s5B, "s5S": s5S, "s5C": s5C, "s5d": s5d}


def make_in_maps(inp, cores=range(NCORES)):
    f = np.float32
    consts = {"ident": np.eye(128, dtype=f),
              "iota": np.ascontiguousarray(np.broadcast_to(np.arange(1024, dtype=f)[None, :], (128, 1024)))}
    r_ = np.arange(128)
    same = (r_[:, None] // 64) == (r_[None, :] // 64)
    s_, t_ = r_[:, None], r_[None, :]
    consts["tri"] = np.stack([same & (s_ <= t_), same & (s_ > t_), same & (s_ >= t_), same & (s_ < t_)]).astype(f)
    r6 = np.arange(64)
    consts["mask"] = np.stack([r6[:, None] <= r6[None, :], r6[:, None] >= r6[None, :]]).astype(f)
    consts["hg_norm_w"] = inp["hg_norm_w"]
    consts["s5_glu_w"] = inp["s5_glu_w"][0]
    consts["glub"] = np.ascontiguousarray(inp["s5_glu_b"][0].reshape(16, 128).T)
    consts["w_branch_a"] = inp["w_branch_a"][0]
    consts["w_branch_b"] = inp["w_branch_b"][0]
    consts["w_out"] = inp["w_out"][0]
    consts["norm_ffn_w"] = inp["norm_ffn_w"]
    consts["rw"] = np.concatenate([inp["router_group_w"][0], inp["router_expert_w"][0]], axis=1)
    consts["rb"] = np.concatenate([inp["router_group_b"][0], inp["router_expert_b"][0]])[None, :]
    consts["gcap"] = (np.arange(8) * 384).astype(f)[None, :]
    consts["tris"] = (r_[:, None] < r_[None, :]).astype(f)
    consts["expert_w1"] = inp["expert_w1"][0]
    consts["expert_w3"] = inp["expert_w3"][0]
    consts["expert_w2"] = inp["expert_w2"][0]
    consts["norm_final_w"] = inp["norm_final_w"][None, :]
    w_in_e = np.ascontiguousarray(inp["w_in"][0])
    w_in_o = np.concatenate([w_in_e[:, :6144], w_in_e[:, 8192:10240], w_in_e[:, 6144:8192], w_in_e[:, 10240:]], axis=1)
    par = []
    for odd in (0, 1):
        dd = [1, 0] if odd else [0, 1]
        m = dict(consts)
        m["w_in"] = w_in_o if odd else w_in_e
        m["norm_mix_w"] = inp["norm_mix_w"]
        m["lbg"] = np.ascontiguousarray(inp["hg_lb_gamma"][dd])
        m.update(_prep_s5(inp, dd))
        par.append(m)
    maps = []
    meta = inp["meta_tokens"]
    z16 = np.zeros_like(meta)
    for cidx in cores:
        b, odd = cidx // 2, cidx % 2
        x = inp["x"][b]
        m = dict(par[odd])
        if not odd:
            m["x_own"] = np.ascontiguousarray(x[:TOWN]); m["x_for"] = np.ascontiguousarray(x[TOWN:])
            m["x_pre"] = np.concatenate([meta, z16], 0)
        else:
            m["x_own"] = np.ascontiguousarray(x[TOWN:][::-1]); m["x_for"] = np.ascontiguousarray(x[:TOWN][::-1])
            m["x_pre"] = np.concatenate([z16, np.ascontiguousarray(meta[::-1])], 0)
        maps.append(m)
    return maps


def kernel(**inputs):
    inp = {k: np.asarray(v) for k, v in inputs.items()}
    nc = build()
    maps = make_in_maps(inp)
    res = run_bass_kernel_spmd(nc, maps, core_ids=list(range(NCORES)))
    outp = np.zeros((4, 4096, D), np.float32)
    for cidx in range(NCORES):
        b, odd = cidx // 2, cidx % 2
        o = res.results[cidx]["out"]
        if not odd:
            outp[b, :TOWN] = o
        else:
            outp[b, TOWN:] = o[::-1]
    return outp
```
